# Optimizing a Trainium2 kernel written in Bass

```python
import math
import jax, jax.numpy as jnp
from jax import lax
import numpy as np

D_MODEL = 1024
BATCH = 4
SEQ = 4096
DEPTH = 4

N_MIXERS = 4
GRID_W = 64
FNET_GROUPS = 4
ATTN_Q_HEADS = 8
ATTN_KV_HEADS = 4
ATTN_HEAD_DIM = D_MODEL // ATTN_Q_HEADS
ATTN_Q_BLOCK = 128
ROPE_THETA = 10000.0
S5_GROUP_CH = 16
S5_GROUPS = D_MODEL // S5_GROUP_CH
S5_STATE = 64
HG_HEAD_DIM = 128
HG_HEADS = D_MODEL // HG_HEAD_DIM
HG_CHUNK = 64
N_EXPERTS = 32
TOP_K = 4
D_EXPERT = D_MODEL
SWIGLU_LIMIT = 7.0
SWIGLU_ALPHA = 1.702
MOE_BLOCK = 128
LN_EPS = 1e-5
RMS_EPS = 1e-6
DEEPNORM_ALPHA = (2 * DEPTH) ** 0.25
DEEPNORM_BETA = (8 * DEPTH) ** -0.25

kernel_name = "hybrid_interleaved_fnet_gqa_s5_hgrn2_moe_encoder"

F32 = jnp.float32


def _layernorm(x):
    xf = x.astype(F32)
    mu = jnp.mean(xf, -1, keepdims=True)
    var = jnp.mean(jnp.square(xf - mu), -1, keepdims=True)
    return (xf - mu) * lax.rsqrt(var + LN_EPS)


def _post_norm(z, g, b):
    return (_layernorm(z) * g.astype(F32) + b.astype(F32)).astype(z.dtype)


def _rmsnorm(x, g):
    xf = x.astype(F32)
    y = xf * lax.rsqrt(jnp.mean(jnp.square(xf), -1, keepdims=True) + RMS_EPS)
    return (y * g.astype(F32)).astype(x.dtype)


def _axial_rope_tables(L):
    rows = L // GRID_W
    row = jnp.broadcast_to(jnp.arange(rows)[:, None], (rows, GRID_W)).reshape(L).astype(F32)
    col = jnp.broadcast_to(jnp.arange(GRID_W)[None, :], (rows, GRID_W)).reshape(L).astype(F32)
    axis_dim = ATTN_HEAD_DIM // 2
    inv_freq = ROPE_THETA ** (-jnp.arange(0, axis_dim, 2, dtype=F32) / axis_dim)
    ang_r = (row[:, None] * inv_freq)[:, None, :]
    ang_c = (col[:, None] * inv_freq)[:, None, :]
    return (jnp.cos(ang_r), jnp.sin(ang_r), jnp.cos(ang_c), jnp.sin(ang_c))


def _rot_half(xp, cos, sin):
    x1, x2 = jnp.split(xp, 2, axis=-1)
    return jnp.concatenate([x1 * cos - x2 * sin, x2 * cos + x1 * sin], axis=-1)


def _apply_axial_rope(x, rope):
    cr, sr, cc, sc = rope
    xf = x.astype(F32)
    half = ATTN_HEAD_DIM // 2
    out = jnp.concatenate([_rot_half(xf[..., :half], cr, sr), _rot_half(xf[..., half:], cc, sc)], axis=-1)
    return out.astype(x.dtype)


def _fnet_mixer(h, w_out, b_out):
    B, L, D = h.shape
    hg = h.astype(F32).reshape(B, L, FNET_GROUPS, D // FNET_GROUPS)
    mixed = jnp.fft.fft2(hg, axes=(1, 3), norm="ortho").real
    return mixed.reshape(B, L, D).astype(h.dtype) @ w_out + b_out


def _attention_mixer(h, w_qkv, q_gain, k_gain, w_out, rope):
    B, L, D = h.shape
    HQ, HK, DH = ATTN_Q_HEADS, ATTN_KV_HEADS, ATTN_HEAD_DIM
    GQ = HQ // HK
    qkv = h @ w_qkv
    q = qkv[..., :HQ * DH].reshape(B, L, HQ, DH)
    k = qkv[..., HQ * DH:(HQ + HK) * DH].reshape(B, L, HK, DH)
    v = qkv[..., (HQ + HK) * DH:].reshape(B, L, HK, DH)
    q = _apply_axial_rope(_rmsnorm(q, q_gain), rope)
    k = _apply_axial_rope(_rmsnorm(k, k_gain), rope)
    q = q.reshape(B, L, HK, GQ, DH).transpose(0, 2, 3, 1, 4)
    k = k.transpose(0, 2, 1, 3)
    v = v.transpose(0, 2, 1, 3)
    nblk = L // ATTN_Q_BLOCK
    qb = q.reshape(B, HK, GQ, nblk, ATTN_Q_BLOCK, DH).transpose(3, 0, 1, 2, 4, 5)
    scale = DH ** -0.5

    def block(qi):
        s = jnp.einsum('bkgqd,bksd->bkgqs', qi, k).astype(F32) * scale
        p = jax.nn.softmax(s, axis=-1).astype(v.dtype)
        return jnp.einsum('bkgqs,bksd->bkgqd', p, v)

    o = lax.map(block, qb)
    o = o.transpose(1, 0, 4, 2, 3, 5).reshape(B, L, HQ * DH)
    return o @ w_out


def _s5_combine(e1, e2):
    a1, b1 = e1
    a2, b2 = e2
    return a1 * a2, a2 * b1 + b2


def _s5_mixer(h, a_re, a_im, log_dt, b_re, b_im, c_re, c_im, d, w_glu, w_out):
    B, L, D = h.shape
    hf = h.astype(F32)
    u = hf.reshape(B, L, S5_GROUPS, S5_GROUP_CH).astype(jnp.complex64)
    y = d.astype(F32) * hf
    for direction in range(2):
        lam = lax.complex(a_re[direction].astype(F32), a_im[direction].astype(F32))
        dt = jnp.exp(log_dt[direction].astype(F32))[:, None]
        a_bar = jnp.exp(lam * dt)
        bmat = lax.complex(b_re[direction].astype(F32), b_im[direction].astype(F32))
        b_bar = ((a_bar - 1.0) / lam)[..., None] * bmat
        bu = jnp.einsum('blgc,gpc->blgp', u, b_bar)
        a_seq = jnp.broadcast_to(a_bar[None, None], (1, L, S5_GROUPS, S5_STATE))
        _, states = lax.associative_scan(_s5_combine, (a_seq, bu), axis=1, reverse=(direction == 1))
        cmat = lax.complex(c_re[direction].astype(F32), c_im[direction].astype(F32))
        y = y + jnp.einsum('blgp,gcp->blgc', states, cmat).real.reshape(B, L, D)
    y = jax.nn.gelu(y).astype(h.dtype)
    y = y * jax.nn.sigmoid(y @ w_glu)
    return y @ w_out


def _hgrn2_chunk_scan(q, k, v, log_f):
    B, H, L, dk = q.shape
    dv = v.shape[-1]
    C = HG_CHUNK
    N = L // C
    q, k, v, log_f = [t.reshape(B, H, N, C, t.shape[-1]) for t in (q, k, v, log_f)]
    b = jnp.cumsum(log_f, axis=3)
    b_last = b[:, :, :, -1:, :]
    q_dec = q * jnp.exp(b)
    att = jnp.einsum('bhncd,bhnsd->bhncs', q_dec, k * jnp.exp(-b))
    att = jnp.where(jnp.tril(jnp.ones((C, C), bool)), att, 0.0)
    o_intra = jnp.einsum('bhncs,bhnsv->bhncv', att, v)
    kv_chunk = jnp.einsum('bhncd,bhncv->bhndv', k * jnp.exp(b_last - b), v)
    decay = jnp.exp(b_last[:, :, :, 0, :])

    def step(S, inp):
        dec, kv = inp
        return dec[..., None] * S + kv, S

    S0 = jnp.zeros((B, H, dk, dv), F32)
    _, S_prev = lax.scan(step, S0, (jnp.moveaxis(decay, 2, 0), jnp.moveaxis(kv_chunk, 2, 0)))
    o_inter = jnp.einsum('bhncd,nbhdv->bhncv', q_dec, S_prev)
    return (o_intra + o_inter).reshape(B, H, L, dv)


def _hgrn2_mixer(h, w_in, lb, norm_g, w_out):
    B, L, D = h.shape
    q, f_fw, f_bw, inp, g = jnp.split(h @ w_in, 5, axis=-1)

    def heads(t):
        return t.reshape(B, L, HG_HEADS, HG_HEAD_DIM).transpose(0, 2, 1, 3).astype(F32)

    lbh = lb.astype(F32).reshape(HG_HEADS, 1, HG_HEAD_DIM)

    def gates(fraw):
        fr = heads(fraw)
        log_f = jnp.log(lbh + (1.0 - lbh) * jax.nn.sigmoid(fr))
        k = (1.0 - lbh) * jax.nn.sigmoid(-fr)
        return k, log_f

    qh = jax.nn.silu(heads(q))
    vh = heads(inp)
    k_fw, lf_fw = gates(f_fw)
    k_bw, lf_bw = gates(f_bw)
    flip = lambda t: jnp.flip(t, axis=2)
    o_fw = _hgrn2_chunk_scan(qh, k_fw, vh, lf_fw)
    o_bw = flip(_hgrn2_chunk_scan(flip(qh), flip(k_bw), flip(vh), flip(lf_bw)))
    o = (o_fw + o_bw).transpose(0, 2, 1, 3)
    o = _rmsnorm(o, norm_g.reshape(HG_HEADS, HG_HEAD_DIM)).reshape(B, L, D).astype(h.dtype)
    return (o * jax.nn.silu(g)) @ w_out


def _moe(h, w_r, b_r, w_gu, b_gu, w_down, b_down):
    B, L, D = h.shape
    N = B * L
    t = h.reshape(N, D)
    logits = (t @ w_r + b_r).astype(F32)
    top_v, top_i = lax.top_k(logits, TOP_K)
    gates = jax.nn.softmax(top_v, axis=-1)
    e_flat = top_i.reshape(-1)
    g_flat = gates.reshape(-1)
    M = N * TOP_K
    tok = jnp.arange(M, dtype=jnp.int32) // TOP_K
    order = jnp.argsort(e_flat, stable=True)
    e_s, tok_s, g_s = e_flat[order], tok[order], g_flat[order]
    gs = jnp.bincount(e_flat, length=N_EXPERTS)
    start = jnp.cumsum(gs) - gs
    ps = ((gs + MOE_BLOCK - 1) // MOE_BLOCK) * MOE_BLOCK
    pend = jnp.cumsum(ps)
    pstart = pend - ps
    dest = pstart[e_s] + jnp.arange(M, dtype=jnp.int32) - start[e_s]
    cap = M + N_EXPERTS * MOE_BLOCK
    nb = cap // MOE_BLOCK
    buf = jnp.zeros((cap, D), t.dtype).at[dest].set(t[tok_s])
    blk_e = jnp.clip(jnp.searchsorted(pend, jnp.arange(nb) * MOE_BLOCK, side='right'), 0, N_EXPERTS - 1)

    def run_block(args):
        xb, e = args
        hgu = xb @ w_gu[e] + b_gu[e]
        gate, up = jnp.split(hgu, 2, axis=-1)
        gate = jnp.minimum(gate, SWIGLU_LIMIT)
        up = jnp.clip(up, -SWIGLU_LIMIT, SWIGLU_LIMIT)
        act = (up + 1.0) * gate * jax.nn.sigmoid(SWIGLU_ALPHA * gate)
        return act @ w_down[e] + b_down[e]

    y = lax.map(run_block, (buf.reshape(nb, MOE_BLOCK, D), blk_e)).reshape(cap, D)[dest]
    out = jnp.zeros((N, D), h.dtype).at[tok_s].add(y * g_s[:, None].astype(y.dtype))
    return out.reshape(B, L, D)


def setup_inputs(seed: int = 0) -> dict:
    key = jax.random.key(seed)
    ks = list(jax.random.split(key, 48))
    D, E, F = D_MODEL, N_EXPERTS, D_EXPERT
    G, P, CH = S5_GROUPS, S5_STATE, S5_GROUP_CH

    def nrm(shape, scale):
        return jax.random.normal(ks.pop(), shape, F32) * scale

    nA, nB, nC, nD = [len(range(m, DEPTH, N_MIXERS)) for m in range(N_MIXERS)]
    w_std = D ** -0.5
    out_std = w_std * DEEPNORM_BETA
    qkv_cols = (ATTN_Q_HEADS + 2 * ATTN_KV_HEADS) * ATTN_HEAD_DIM
    n_idx = jnp.arange(P, dtype=F32)
    return {
        "x": nrm((BATCH, SEQ, D), 1.0),
        "c": nrm((BATCH, D), 1.0),
        "ada_w": nrm((DEPTH, D, 6 * D), 0.5 * w_std),
        "ada_b": nrm((DEPTH, 6 * D), 0.02),
        "post_ln_g": 1.0 + nrm((DEPTH, 2, D), 0.02),
        "post_ln_b": nrm((DEPTH, 2, D), 0.02),
        "fnet_w_out": nrm((nA, D, D), out_std),
        "fnet_b_out": nrm((nA, D), 0.02),
        "attn_w_qkv": nrm((nB, D, qkv_cols), w_std),
        "attn_q_norm": 1.0 + nrm((nB, ATTN_HEAD_DIM), 0.02),
        "attn_k_norm": 1.0 + nrm((nB, ATTN_HEAD_DIM), 0.02),
        "attn_w_out": nrm((nB, ATTN_Q_HEADS * ATTN_HEAD_DIM, D), out_std),
        "s5_a_re": -0.5 + nrm((nC, 2, G, P), 0.01),
        "s5_a_im": jnp.pi * n_idx + nrm((nC, 2, G, P), 0.01),
        "s5_log_dt": jax.random.uniform(ks.pop(), (nC, 2, G), F32, math.log(1e-3), math.log(1e-1)),
        "s5_b_re": nrm((nC, 2, G, P, CH), (2 * CH) ** -0.5),
        "s5_b_im": nrm((nC, 2, G, P, CH), (2 * CH) ** -0.5),
        "s5_c_re": nrm((nC, 2, G, CH, P), P ** -0.5),
        "s5_c_im": nrm((nC, 2, G, CH, P), P ** -0.5),
        "s5_d": nrm((nC, D), 1.0),
        "s5_w_glu": nrm((nC, D, D), w_std),
        "s5_w_out": nrm((nC, D, D), out_std),
        "hg_w_in": nrm((nD, D, 5 * D), w_std),
        "hg_lb": nrm((DEPTH, D), 0.1),
        "hg_norm": 1.0 + nrm((nD, D), 0.02),
        "hg_w_out": nrm((nD, D, D), out_std),
        "moe_w_router": nrm((DEPTH, D, E), w_std),
        "moe_b_router": nrm((DEPTH, E), 0.01),
        "moe_w_gu": nrm((DEPTH, E, D, 2 * F), w_std),
        "moe_b_gu": nrm((DEPTH, E, 2 * F), 0.02),
        "moe_w_down": nrm((DEPTH, E, F, D), F ** -0.5 * DEEPNORM_BETA),
        "moe_b_down": nrm((DEPTH, E, D), 0.02),
    }


def reference(x, c, ada_w, ada_b, post_ln_g, post_ln_b, fnet_w_out, fnet_b_out, attn_w_qkv, attn_q_norm, attn_k_norm, attn_w_out, s5_a_re, s5_a_im, s5_log_dt, s5_b_re, s5_b_im, s5_c_re, s5_c_im, s5_d, s5_w_glu, s5_w_out, hg_w_in, hg_lb, hg_norm, hg_w_out, moe_w_router, moe_b_router, moe_w_gu, moe_b_gu, moe_w_down, moe_b_down):
    dt = x.dtype
    L = x.shape[1]
    rope = _axial_rope_tables(L)
    lb_soft = jax.nn.softmax(hg_lb.astype(F32), axis=0)
    lb_all = jnp.cumsum(lb_soft, axis=0) - lb_soft[0]
    cond = jax.nn.silu(c)
    for i in range(DEPTH):
        mod = (cond @ ada_w[i] + ada_b[i])[:, None, :]
        sh1, sc1, g1, sh2, sc2, g2 = jnp.split(mod, 6, axis=-1)
        h = (_layernorm(x) * (1.0 + sc1) + sh1).astype(dt)
        m, j = i % N_MIXERS, i // N_MIXERS
        if m == 0:
            y = _fnet_mixer(h, fnet_w_out[j], fnet_b_out[j])
        elif m == 1:
            y = _attention_mixer(h, attn_w_qkv[j], attn_q_norm[j], attn_k_norm[j], attn_w_out[j], rope)
        elif m == 2:
            y = _s5_mixer(h, s5_a_re[j], s5_a_im[j], s5_log_dt[j], s5_b_re[j], s5_b_im[j], s5_c_re[j], s5_c_im[j], s5_d[j], s5_w_glu[j], s5_w_out[j])
        else:
            y = _hgrn2_mixer(h, hg_w_in[j], lb_all[i], hg_norm[j], hg_w_out[j])
        x = _post_norm(DEEPNORM_ALPHA * x + g1 * y, post_ln_g[i, 0], post_ln_b[i, 0])
        h = (_layernorm(x) * (1.0 + sc2) + sh2).astype(dt)
        y = _moe(h, moe_w_router[i], moe_b_router[i], moe_w_gu[i], moe_b_gu[i], moe_w_down[i], moe_b_down[i])
        x = _post_norm(DEEPNORM_ALPHA * x + g2 * y, post_ln_g[i, 1], post_ln_b[i, 1])
    return x
```

```python
from contextlib import ExitStack, contextmanager
import math
import numpy as np
import ml_dtypes
import concourse.bass as bass
import concourse.mybir as mybir
from concourse.bass_utils import run_bass_kernel_spmd

F32 = mybir.dt.float32
BF16 = mybir.dt.bfloat16
I32 = mybir.dt.int32
ALU = mybir.AluOpType
ACT = mybir.ActivationFunctionType
AX = mybir.AxisListType

NDMASEM = 8
D_MODEL = 1024
SEQ = 4096
HALF = 2048
NEXP = 32
DEPTH = 4
ALPHA = (2 * DEPTH) ** 0.25
LN_EPS = 1e-5
RMS_EPS = 1e-6


class Prog:
    ENGS = ("pe", "dve", "act", "pool", "sp")

    def __init__(self, nc):
        self.nc = nc
        self.ops = []
        self.last_w = {}
        self.readers = {}
        self.gstack = ExitStack()
        self.stacks = [self.gstack]
        self.sems = {e: self.gstack.enter_context(nc.semaphore("s_" + e)) for e in ("pe", "dve", "act", "pool")}
        self.dsems = {e: [self.gstack.enter_context(nc.semaphore("d_%s%d" % (e, j))) for j in range(NDMASEM)]
                      for e in ("sp", "act", "pool")}
        self.cnt = {e: 0 for e in self.ENGS}
        self.dcnt = {e: 0 for e in self.ENGS}
        self.waited = {e: {} for e in self.ENGS}
        self.total_ops = 0

    def sbuf(self, name, shape, dtype):
        self.uid = getattr(self, "uid", 0) + 1
        return self.stacks[-1].enter_context(self.nc.sbuf_tensor("sb%d_%s" % (self.uid, name), list(shape), dtype))

    def psum(self, name, shape, dtype=F32):
        self.uid = getattr(self, "uid", 0) + 1
        return self.stacks[-1].enter_context(self.nc.psum_tensor("ps%d_%s" % (self.uid, name), list(shape), dtype))

    @contextmanager
    def scope(self):
        st = ExitStack()
        self.stacks.append(st)
        yield
        self.flush()
        self.stacks.pop()
        st.close()

    def op(self, eng, fn, r=(), w=(), dma=False):
        i = len(self.ops)
        deps = set()
        for k in r:
            if k in self.last_w:
                deps.add(self.last_w[k])
        for k in w:
            if k in self.last_w:
                deps.add(self.last_w[k])
            for q in self.readers.get(k, ()):
                deps.add(q)
        for k in w:
            self.last_w[k] = i
            self.readers[k] = []
        for k in r:
            if k not in w:
                self.readers.setdefault(k, []).append(i)
        deps.discard(i)
        self.ops.append(dict(eng=eng, fn=fn, deps=deps, dma=dma))
        return i

    def dma(self, q, out, in_, r=(), w=(), **kw):
        def fn(e):
            return e.dma_start(out=out, in_=in_, **kw)
        return self.op(q, fn, r=r, w=w, dma=True)

    def mm(self, out, lhsT, rhs, start=True, stop=True, r=(), w=()):
        def fn(e):
            return e.matmul(out, lhsT, rhs, start=start, stop=stop)
        rr = list(r)
        if not start:
            rr = rr + list(w)
        return self.op("pe", fn, r=rr, w=w)

    def act(self, out, in_, func, r=(), w=(), **kw):
        def fn(e):
            return e.activation(out=out, in_=in_, func=func, **kw)
        return self.op("act", fn, r=r, w=w)

    def tt(self, eng, out, in0, in1, op, r=(), w=()):
        def fn(e):
            return e.tensor_tensor(out=out, in0=in0, in1=in1, op=op)
        return self.op(eng, fn, r=r, w=w)

    def ts(self, eng, out, in0, s1, op0, s2=None, op1=None, r=(), w=(), **kw):
        def fn(e):
            if op1 is None:
                return e.tensor_scalar(out=out, in0=in0, scalar1=s1, scalar2=None, op0=op0, **kw)
            return e.tensor_scalar(out=out, in0=in0, scalar1=s1, scalar2=s2, op0=op0, op1=op1, **kw)
        return self.op(eng, fn, r=r, w=w)

    def stt(self, eng, out, in0, scalar, in1, op0, op1, r=(), w=()):
        def fn(e):
            return e.scalar_tensor_tensor(out=out, in0=in0, scalar=scalar, in1=in1, op0=op0, op1=op1)
        return self.op(eng, fn, r=r, w=w)

    def copy(self, eng, out, in_, r=(), w=()):
        if eng == "act":
            def fn(e):
                return e.copy(out=out, in_=in_)
        else:
            def fn(e):
                return e.tensor_copy(out=out, in_=in_)
        return self.op(eng, fn, r=r, w=w)

    def memset(self, eng, ap, val, w=()):
        def fn(e):
            return e.memset(ap, val)
        return self.op(eng, fn, w=w)

    def flush(self):
        nc = self.nc
        ops = self.ops
        if not ops:
            return
        engs = {}
        for i, o in enumerate(ops):
            engs.setdefault(o["eng"], []).append(i)

        def skip(p, o):
            return (not p["dma"]) and (not o["dma"]) and p["eng"] == o["eng"] == "pe"
        needed = set()
        for i, o in enumerate(ops):
            best = {}
            keep = set()
            for d in o["deps"]:
                pd = ops[d]
                if skip(pd, o):
                    continue
                if pd["dma"]:
                    keep.add(d)
                else:
                    if pd["eng"] not in best or best[pd["eng"]] < d:
                        best[pd["eng"]] = d
            keep.update(best.values())
            o["deps"] = keep
            needed.update(keep)
        for i, o in enumerate(ops):
            e = o["eng"]
            o["prewait"] = None
            if o["dma"]:
                k = self.dcnt[e]
                self.dcnt[e] += 1
                s = self.dsems[e][k % NDMASEM]
                o["sig"] = (s, 16 * (k // NDMASEM + 1), 16)
                if k >= NDMASEM:
                    o["prewait"] = (s, 16 * (k // NDMASEM))
            elif i in needed:
                self.cnt[e] += 1
                o["sig"] = (self.sems[e], self.cnt[e], 1)
            else:
                o["sig"] = None
        for e, lst in engs.items():
            waited = self.waited[e]
            for i in lst:
                o = ops[i]
                ws = {}
                if o["prewait"] is not None:
                    s_, v = o["prewait"]
                    ws[id(s_)] = (s_, v)
                for d in o["deps"]:
                    p = ops[d]
                    if skip(p, o):
                        continue
                    s_, v, _ = p["sig"]
                    if id(s_) not in ws or ws[id(s_)][1] < v:
                        ws[id(s_)] = (s_, v)
                o["waits"] = []
                for kk, (s_, v) in ws.items():
                    if waited.get(kk, 0) < v:
                        waited[kk] = v
                        o["waits"].append((s_, v))
        finals = {}
        for e in engs:
            if e in self.dsems:
                n = self.dcnt[e]
                fl = []
                for j in range(NDMASEM):
                    c = (n - j + NDMASEM - 1) // NDMASEM if n > j else 0
                    if c > 0 and self.waited[e].get(id(self.dsems[e][j]), 0) < 16 * c:
                        self.waited[e][id(self.dsems[e][j])] = 16 * c
                        fl.append((self.dsems[e][j], 16 * c))
                finals[e] = fl
        self.total_ops += len(ops)
        with nc.Block() as block:
            def mk(e):
                lst = engs[e]

                def body(eng):
                    for i in lst:
                        o = ops[i]
                        for s, v in o["waits"]:
                            eng.wait_ge(s, v)
                        inst = o["fn"](eng)
                        if o["sig"] is not None:
                            s, _, inc = o["sig"]
                            inst.then_inc(s, inc)
                    for s, v in finals.get(e, ()):
                        eng.wait_ge(s, v)
                return body
            reg = {"pe": block.tensor, "dve": block.vector, "act": block.scalar,
                   "pool": block.gpsimd, "sp": block.sync}
            for e in engs:
                reg[e](mk(e))
        nc.all_engine_barrier()
        self.ops = []
        self.last_w = {}
        self.readers = {}

    def new_epoch(self):
        self.flush()
        self.epoch = getattr(self, "epoch", 0) + 1
        self.sems = {e: self.gstack.enter_context(self.nc.semaphore("s%d_%s" % (self.epoch, e)))
                     for e in ("pe", "dve", "act", "pool")}
        for e in ("pe", "dve", "act", "pool"):
            self.cnt[e] = 0

    def finish(self):
        self.flush()
        self.gstack.close()


class Rot:
    def __init__(self, p, name, shape, dtype, n, psum=False):
        self.t = [(p.psum if psum else p.sbuf)("%s%d" % (name, i), shape, dtype) for i in range(n)]
        self.name = name
        self.i = -1

    def next(self):
        self.i += 1
        j = self.i % len(self.t)
        return self.t[j], (self.name, j)


class Ctx:
    pass


def setup_consts(c):
    p = c.p
    c.ident = p.sbuf("ident", [128, 128], F32)
    c.identb = p.sbuf("identb", [128, 128], BF16)
    c.ones = p.sbuf("ones", [128, 128], F32)
    p.memset("dve", c.ident[:], 0.0, w=["ident"])
    p.memset("dve", c.ones[:], 1.0, w=["ones"])

    def aff(e):
        return e.affine_select(out=c.ident[:], in_=c.ident[:], pattern=[[-1, 128]], compare_op=ALU.not_equal,
                               fill=1.0, base=0, channel_multiplier=1)
    p.op("pool", aff, r=["ident"], w=["ident"])
    p.copy("dve", c.identb[:], c.ident[:], r=["ident"], w=["identb"])
    p.flush()


def phase_mod(c):
    p, D = c.p, c.D
    with p.scope():
        cT = p.sbuf("cT", [128, 8], F32)
        cs = p.sbuf("cs", [128, 8], F32)
        crep = p.sbuf("crep", [128, 8, 128], F32)
        abias = p.sbuf("abias", [1, 6144], F32)
        aw = Rot(p, "aw", [128, 8, 512], F32, 2)
        mt = Rot(p, "mt", [128, 512], F32, 2)
        ps = Rot(p, "psm", [128, 512], F32, 2, psum=True)
        p.dma("sp", cT[:], D["cT"][:, :], w=["cT"])
        p.dma("sp", abias[:], D["ada_b"][:, :], w=["abias"])
        p.act(cs[:], cT[:], ACT.Silu, r=["cT"], w=["cs"])
        for k in range(8):
            p.ts("dve", crep[:, k, :], c.ones[:, :], cs[:, k:k + 1], ALU.mult, r=["cs"], w=[("crep", k)])
        for n in range(12):
            awt, awk = aw.next()
            pst, psk = ps.next()
            mtt, mtk = mt.next()
            p.dma("sp" if n % 2 == 0 else "act", awt[:],
                  D["ada_w"][:, n * 512:(n + 1) * 512].rearrange("(c p) n -> p c n", p=128), w=[awk])
            for k in range(8):
                p.mm(pst[:], crep[:, k, :], awt[:, k, :], start=(k == 0), stop=False, r=[("crep", k), awk], w=[psk])
            p.mm(pst[:], c.ones[0:1, :], abias[0:1, n * 512:(n + 1) * 512], start=False, stop=True, r=["abias"], w=[psk])
            if n in (2, 3, 8, 9):
                p.ts("dve", mtt[:], pst[:], 1.0, ALU.add, r=[psk], w=[mtk])
            else:
                p.copy("dve", mtt[:], pst[:], r=[psk], w=[mtk])
            p.dma("sp", c.modd[:, n * 512:(n + 1) * 512], mtt[:], r=[mtk], w=[("modd", n)])


class LN:
    def __init__(self, p, name, nbuf=2):
        self.p = p
        self.st = Rot(p, name + "_st", [128, 2, 6], F32, nbuf)
        self.mv = Rot(p, name + "_mv", [128, 2], F32, nbuf)
        self.rs = Rot(p, name + "_rs", [128, 1], F32, nbuf)
        self.xn = Rot(p, name + "_xn", [128, 1024], F32, nbuf)

    def __call__(self, xin, xk, out, outk, scale, shift, ck, eng2="pool", extra_r=()):
        p = self.p
        st, stk = self.st.next()
        mv, mvk = self.mv.next()
        rs, rsk = self.rs.next()
        xn, xnk = self.xn.next()
        for hh in range(2):
            def f(e, hh=hh):
                return e.bn_stats(out=st[:, hh, :], in_=xin[:, hh * 512:(hh + 1) * 512])
            p.op("dve", f, r=[xk], w=[(stk, hh)])

        def g(e):
            return e.bn_aggr(out=mv[:], in_=st[:])
        p.op("dve", g, r=[(stk, 0), (stk, 1)], w=[mvk])
        p.ts("dve", rs[:], mv[:, 1:2], LN_EPS, ALU.add, r=[mvk], w=[rsk])
        p.op("act", (lambda e, rs=rs: e.sqrt(out=rs[:], in_=rs[:])), r=[rsk], w=[rsk])
        p.op("dve", (lambda e, rs=rs: e.reciprocal(out=rs[:], in_=rs[:])), r=[rsk], w=[rsk])
        p.ts("dve", xn[:], xin, mv[:, 0:1], ALU.subtract, rs[:, 0:1], ALU.mult, r=[xk, mvk, rsk], w=[xnk])
        p.tt(eng2, xn[:], xn[:], scale, ALU.mult, r=[xnk] + list(ck), w=[xnk])
        p.tt(eng2, out, xn[:], shift, ALU.add, r=[xnk] + list(ck) + list(extra_r), w=[outk])


def phase_post(c, ymix):
    p, D = c.p, c.D
    with p.scope():
        mods = p.sbuf("modsB", [128, 3, 1024], F32)
        pl = p.sbuf("plB", [128, 2, 1024], F32)
        wr = p.sbuf("wr", [128, 8, 32], F32)
        br = p.sbuf("br", [1, 32], F32)
        p.dma("sp", mods[:, 0, :], c.modd[:, 2048:3072], w=["mods"])
        p.dma("sp", mods[:, 1, :], c.modd[:, 3072:4096], w=["mods"])
        p.dma("sp", mods[:, 2, :], c.modd[:, 4096:5120], w=["mods"])
        p.dma("act", pl[:, 0, :], D["pln"][0], w=["pl"])
        p.dma("act", pl[:, 1, :], D["pln"][1], w=["pl"])
        p.dma("act", wr[:], D["w_r"].rearrange("(c p) n -> p c n", p=128), w=["wr"])
        p.dma("act", br[:], D["b_r"][:, :], w=["br"])
        xt = Rot(p, "xtB", [128, 1024], F32, 2)
        yt = Rot(p, "ytB", [128, 1024], F32, 2)
        x1 = Rot(p, "x1B", [128, 1024], F32, 2)
        h2 = Rot(p, "h2B", [128, 1024], F32, 2)
        hTf = Rot(p, "hTf", [128, 8, 128], F32, 2)
        hTb = Rot(p, "hTb", [128, 8, 128], BF16, 2)
        pst = Rot(p, "pstB", [128, 4, 128], F32, 4, psum=True)
        psl = Rot(p, "pslB", [128, 512], F32, 2, psum=True)
        lg = Rot(p, "lg", [128, 32], F32, 2)
        m8 = Rot(p, "m8", [128, 8], F32, 2)
        nm = Rot(p, "nm", [128, 1], F32, 2)
        msk = Rot(p, "msk", [128, 32], F32, 2)
        ex = Rot(p, "ex", [128, 32], F32, 2)
        sm = Rot(p, "sm", [128, 1], F32, 2)
        gt = Rot(p, "gt", [128, 32], F32, 2)
        ln1 = LN(p, "lnB1")
        ln2 = LN(p, "lnB2")
        for t in range(16):
            x, xk = xt.next()
            y, yk = yt.next()
            p.dma("sp", x[:], D["xp"][t * 128:(t + 1) * 128, :], w=[xk])
            p.dma("act", y[:], ymix[t * 128:(t + 1) * 128, :], w=[yk])
            p.tt("pool", y[:], y[:], mods[:, 0, :], ALU.mult, r=[yk, "mods"], w=[yk])
            p.stt("dve", y[:], x[:], ALPHA, y[:], ALU.mult, ALU.add, r=[xk, yk], w=[yk])
            a, ak = x1.next()
            ln1(y[:], yk, a[:], ak, pl[:, 0, :], pl[:, 1, :], ["pl"])
            p.dma("sp", c.x1buf[t * 128:(t + 1) * 128, :], a[:], r=[ak], w=[("x1buf", t)])
            import os
            KP = int(os.environ.get("KPOST", "9"))
            if KP < 2:
                continue
            h, hk = h2.next()
            ln2(a[:], ak, h[:], hk, mods[:, 2, :], mods[:, 1, :], ["mods"])
            hf_, hfk = hTf.next()
            hb_, hbk = hTb.next()
            if os.environ.get("KV", "") == "noT":
                continue
            for q in range(2):
                ps, psk = pst.next()
                for k in range(4):
                    p.mm(ps[:, k, :], h[:, (q * 4 + k) * 128:(q * 4 + k + 1) * 128], c.ident[:], r=[hk], w=[(psk, k)])
                pk = [(psk, k) for k in range(4)]
                p.copy("act", hf_[:, q * 4:(q + 1) * 4, :], ps[:], r=pk, w=[(hfk, q)])
                p.copy("pool", hb_[:, q * 4:(q + 1) * 4, :], hf_[:, q * 4:(q + 1) * 4, :], r=[(hfk, q)], w=[(hbk, q)])
            hfk2 = [(hfk, 0), (hfk, 1)]
            hbk2 = [(hbk, 0), (hbk, 1)]
            if os.environ.get("KV", "") != "nodma":
                p.dma("sp", c.h2T[:, :, t * 128:(t + 1) * 128], hb_[:], r=hbk2, w=[("h2T", t)])
            if KP < 3:
                continue
            pl_, plk = psl.next()
            for k in range(8):
                p.mm(pl_[:, 0:32], hf_[:, k, :], wr[:, k, :], start=(k == 0), stop=False, r=hfk2 + ["wr"], w=[plk])
            p.mm(pl_[:, 0:32], c.ones[0:1, :], br[0:1, :], start=False, stop=True, r=["br"], w=[plk])
            l, lk = lg.next()
            p.copy("dve", l[:], pl_[:, 0:32], r=[plk], w=[lk])
            if KP < 4:
                continue
            m, mk_ = m8.next()

            def fmax(e, m=m, l=l):
                return e.max(out=m[:], in_=l[:])
            p.op("dve", fmax, r=[lk], w=[mk_])
            n_, nk = nm.next()
            p.ts("dve", n_[:], m[:, 0:1], -1.0, ALU.mult, r=[mk_], w=[nk])
            ms, msk_ = msk.next()
            p.ts("dve", ms[:], l[:], m[:, 3:4], ALU.is_ge, r=[lk, mk_], w=[msk_])
            e_, ek = ex.next()
            p.act(e_[:], l[:], ACT.Exp, bias=n_[:, 0:1], scale=1.0, r=[lk, nk], w=[ek])
            p.tt("dve", e_[:], e_[:], ms[:], ALU.mult, r=[ek, msk_], w=[ek])
            s_, sk = sm.next()

            def fsum(e, s_=s_, e_=e_):
                return e.reduce_sum(out=s_[:], in_=e_[:], axis=AX.X)
            p.op("dve", fsum, r=[ek], w=[sk])
            g_, gk = gt.next()
            p.op("dve", (lambda e, s_=s_: e.reciprocal(out=s_[:], in_=s_[:])), r=[sk], w=[sk])
            p.ts("dve", g_[:], e_[:], s_[:, 0:1], ALU.mult, r=[ek, sk], w=[gk])
            p.dma("sp", c.gbuf[t], g_[:], r=[gk], w=[("gbuf", t)])


def phase_moe(c):
    p, D = c.p, c.D
    for tp in range(2):
        with p.scope():
            hT = p.sbuf("hTm", [128, 8, 1024], BF16)
            G = p.sbuf("Gm", [128, 8, 32], F32)
            acc = p.sbuf("accm", [128, 8, 1024], F32)
            bgu = p.sbuf("bgu", [128, NEXP, 16], F32)
            p.dma("sp", hT[:], c.h2T[:, :, tp * 1024:(tp + 1) * 1024], w=["hT"])
            p.dma("sp", G[:], c.gbuf[tp * 8:(tp + 1) * 8].rearrange("t p e -> p t e"), w=["G"])
            p.dma("sp", bgu[:], D["b_guT"][:, :, :], w=["bgu"])
            bgu1 = p.sbuf("bgu1", [128, NEXP, 16], F32)
            p.ts("dve", bgu1[:], bgu[:], 1.0, ALU.add, r=["bgu"], w=["bgu1"])
            F7 = 7.0 / (1.0 + math.exp(-1.702 * 7.0))
            with p.scope():
                wgu = Rot(p, "wgu", [128, 8, 2048], BF16, 2)
                wd = Rot(p, "wd", [128, 8, 1024], BF16, 2)
                bd = Rot(p, "bd", [1, 1024], F32, 2)
                psg = Rot(p, "psg", [128, 512], F32, 3, psum=True)
                psu = Rot(p, "psu", [128, 512], F32, 3, psum=True)
                pso = Rot(p, "pso", [128, 512], F32, 2, psum=True)
                gs = Rot(p, "gs", [128, 512], F32, 4)
                sg = Rot(p, "sg", [128, 512], F32, 4)
                us = Rot(p, "us", [128, 512], F32, 4)
                aT = Rot(p, "aT", [128, 8, 512], BF16, 2)
                for e in range(NEXP):
                    wg_, wgk = wgu.next()
                    wd_, wdk = wd.next()
                    bd_, bdk = bd.next()
                    p.dma("pool", wg_[:], D["w_gu"][e].rearrange("(c p) n -> p c n", p=128), w=[wgk])
                    p.dma("pool", wd_[:], D["w_down"][e].rearrange("(c p) n -> p c n", p=128), w=[wdk])
                    p.dma("sp", bd_[:], D["b_down"][e:e + 1, :], w=[bdk])
                    for blk in range(2):
                        a_, aTk = aT.next()
                        for j in range(8):
                            pg, pgk = psg.next()
                            pu, puk = psu.next()
                            for k in range(8):
                                p.mm(pg[:], wg_[:, k, j * 128:(j + 1) * 128], hT[:, k, blk * 512:(blk + 1) * 512],
                                     start=(k == 0), stop=(k == 7), r=[wgk, "hT"], w=[pgk])
                            for k in range(8):
                                p.mm(pu[:], wg_[:, k, 1024 + j * 128:1024 + (j + 1) * 128], hT[:, k, blk * 512:(blk + 1) * 512],
                                     start=(k == 0), stop=(k == 7), r=[wgk, "hT"], w=[puk])
                            g_, gk = gs.next()
                            u_, uk = us.next()
                            p.act(g_[:], pg[:], ACT.Gelu_apprx_sigmoid, bias=bgu[:, e, j:j + 1], scale=1.0, r=[pgk, "bgu"], w=[gk])
                            p.act(u_[:], pu[:], ACT.Identity, bias=bgu1[:, e, 8 + j:9 + j], scale=1.0, r=[puk, "bgu1"], w=[uk])
                            p.ts("dve", u_[:], u_[:], 8.0, ALU.min, -6.0, ALU.max, r=[uk], w=[uk])
                            p.stt("dve", a_[:, j, :], g_[:], F7, u_[:], ALU.min, ALU.mult, r=[gk, uk], w=[(aTk, j)])
                        for tl in range(4):
                            tt_ = blk * 4 + tl
                            for hh in range(2):
                                po, pok = pso.next()
                                for j in range(8):
                                    p.mm(po[:], a_[:, j, tl * 128:(tl + 1) * 128], wd_[:, j, hh * 512:(hh + 1) * 512],
                                         start=(j == 0), stop=False, r=[(aTk, j), wdk], w=[pok])
                                p.mm(po[:], c.ones[0:1, :], bd_[0:1, hh * 512:(hh + 1) * 512], start=False, stop=True,
                                     r=[bdk], w=[pok])
                                dst = acc[:, tt_, hh * 512:(hh + 1) * 512]
                                ak = ("acc", tt_, hh)
                                if e == 0:
                                    p.ts("dve", dst, po[:], G[:, tt_, e:e + 1], ALU.mult, r=[pok, "G"], w=[ak])
                                else:
                                    p.stt("dve", dst, po[:], G[:, tt_, e:e + 1], dst, ALU.mult, ALU.add,
                                          r=[pok, "G", ak], w=[ak])
            g2 = p.sbuf("g2m", [128, 1024], F32)
            pl = p.sbuf("plm", [128, 2, 1024], F32)
            p.dma("sp", g2[:], c.modd[:, 5120:6144], w=["g2"])
            p.dma("sp", pl[:, 0, :], D["pln"][2], w=["plm"])
            p.dma("sp", pl[:, 1, :], D["pln"][3], w=["plm"])
            x1 = Rot(p, "x1m", [128, 1024], F32, 2)
            ot = Rot(p, "otm", [128, 1024], F32, 2)
            ln = LN(p, "lnM")
            for tl in range(8):
                t = tp * 8 + tl
                a, ak = x1.next()
                p.dma("sp", a[:], c.x1buf[t * 128:(t + 1) * 128, :], w=[ak])
                z = acc[:, tl, :]
                zk = [("acc", tl, 0), ("acc", tl, 1)]
                p.tt("pool", z, z, g2[:], ALU.mult, r=zk + ["g2"], w=zk)
                p.stt("dve", a[:], a[:], ALPHA, z, ALU.mult, ALU.add, r=[ak] + zk, w=[ak])
                o, ok = ot.next()
                ln(a[:], ak, o[:], ok, pl[:, 0, :], pl[:, 1, :], ["plm"])
                p.dma("sp", D["out"][t * 128:(t + 1) * 128, :], o[:], r=[ok], w=[("out", t)])


def phase_fnet(c, ymix):
    p, D = c.p, c.D
    with p.scope():
        hall = p.sbuf("hall", [128, 32, 1024], BF16)
        with p.scope():
            mods = p.sbuf("modsA", [128, 2, 1024], F32)
            p.dma("sp", mods[:, 0, :], c.modd[:, 0:1024], w=["mods"])
            p.dma("sp", mods[:, 1, :], c.modd[:, 1024:2048], w=["mods"])
            xt = Rot(p, "xtA", [128, 1024], F32, 3)
            ln = LN(p, "lnA")
            for t in range(32):
                x, xk = xt.next()
                p.dma("sp" if t % 2 == 0 else "act", x[:], D["xp"][t * 128:(t + 1) * 128, :], w=[xk])
                ln(x[:], xk, hall[:, t, :], ("hall", t), mods[:, 1, :], mods[:, 0, :], ["mods"])
        abT = p.sbuf("abT", [128, 2, 8, HALF], BF16)
        with p.scope():
            tab = Rot(p, "tab", [128, 4, 2, 512], BF16, 3)
            psa = Rot(p, "psa", [128, 512], F32, 8, psum=True)
            qi = 0
            for oc in range(4):
                for cp in range(4):
                    accs = [psa.next() for _ in range(4)]
                    for l4 in range(8):
                        tb, tbk = tab.next()
                        p.dma("sp" if qi % 2 == 0 else "act", tb[:], D["dftL"][oc, l4], w=[tbk])
                        qi += 1
                        for li in range(4):
                            lt = l4 * 4 + li
                            for ci in range(2):
                                ch = cp * 2 + ci
                                for cs in range(2):
                                    pt, pk = accs[ci * 2 + cs]
                                    p.mm(pt[:], hall[:, lt, ch * 128:(ch + 1) * 128], tb[:, li, cs, :],
                                         start=(lt == 0), stop=(lt == 31), r=[("hall", lt), tbk], w=[pk])
                    for ci in range(2):
                        ch = cp * 2 + ci
                        for cs in range(2):
                            pt, pk = accs[ci * 2 + cs]
                            dst = abT[:, cs, ch, oc * 512:(oc + 1) * 512]
                            if cs == 0:
                                p.op("act", (lambda e, dst=dst, pt=pt: e.mul(out=dst, in_=pt[:], mul=1.0 / 1024.0)), r=[pk], w=[("abT", cs, ch, oc)])
                            else:
                                p.ts("dve", dst, pt[:], 1.0 / 1024.0, ALU.mult, r=[pk], w=[("abT", cs, ch, oc)])
        with p.scope():
            cc = p.sbuf("ccs", [128, 2, 2, 256], BF16)
            p.dma("sp", cc[:], D["dftC"][:, :, :, :], w=["cc"])
            wo = p.sbuf("woA", [128, 8, 1024], BF16)
            p.dma("pool", wo[:], D["fnet_w"].rearrange("(c p) n -> p c n", p=128), w=["wo"])
            bo = p.sbuf("boA", [1, 1024], F32)
            p.dma("sp", bo[:], D["fnet_b"][:, :], w=["bo"])
            yT = Rot(p, "yTA", [128, 8, 512], BF16, 2)
            psy = Rot(p, "psy", [128, 512], F32, 2, psum=True)
            pso = Rot(p, "psoA", [128, 512], F32, 2, psum=True)
            yo = Rot(p, "yoA", [128, 1024], F32, 2)
            for oc in range(4):
                y_, yk = yT.next()
                for kc in range(8):
                    g, kk = kc // 2, kc % 2
                    ps, psk = psy.next()
                    n = 0
                    for cs in range(2):
                        for cch in range(2):
                            p.mm(ps[:], cc[:, cch, cs, kk * 128:(kk + 1) * 128],
                                 abT[:, cs, g * 2 + cch, oc * 512:(oc + 1) * 512],
                                 start=(n == 0), stop=(n == 3), r=["cc", ("abT", cs, g * 2 + cch, oc)], w=[psk])
                            n += 1
                    if kc % 2 == 0:
                        p.copy("act", y_[:, kc, :], ps[:], r=[psk], w=[(yk, kc)])
                    else:
                        p.copy("dve", y_[:, kc, :], ps[:], r=[psk], w=[(yk, kc)])
                for tl in range(4):
                    t = oc * 4 + tl
                    o, ok = yo.next()
                    for hh in range(2):
                        po, pok = pso.next()
                        for kc in range(8):
                            p.mm(po[:], y_[:, kc, tl * 128:(tl + 1) * 128], wo[:, kc, hh * 512:(hh + 1) * 512],
                                 start=(kc == 0), stop=False, r=[(yk, kc), "wo"], w=[pok])
                        p.mm(po[:], c.ones[0:1, :], bo[0:1, hh * 512:(hh + 1) * 512], start=False, stop=True,
                             r=["bo"], w=[pok])
                        if hh == 0:
                            p.copy("act", o[:, 0:512], po[:], r=[pok], w=[(ok, 0)])
                        else:
                            p.copy("dve", o[:, 512:1024], po[:], r=[pok], w=[(ok, 1)])
                    p.dma("sp", ymix[t * 128:(t + 1) * 128, :], o[:], r=[(ok, 0), (ok, 1)], w=[("ymix", t)])


def build_layer(m):
    nc = bass.Bass("TRN2", target_bir_lowering=False)
    c = Ctx()
    c.nc = nc
    D = {}
    c.D = D

    def din(name, shape, dt=F32):
        D[name] = nc.dram_tensor(name, list(shape), dt, kind="ExternalInput").ap()

    din("xp", [SEQ, D_MODEL])
    din("cT", [128, 8])
    din("ada_w", [D_MODEL, 6 * D_MODEL])
    din("ada_b", [1, 6 * D_MODEL])
    din("pln", [4, 128, D_MODEL])
    din("w_r", [D_MODEL, NEXP])
    din("b_r", [1, NEXP])
    import os
    ph = os.environ.get("KPH", "mod,mix,post,moe").split(",")
    if "moe" in ph:
        din("w_gu", [NEXP, D_MODEL, 2 * D_MODEL])
        din("b_guT", [128, NEXP, 16])
        din("w_down", [NEXP, D_MODEL, D_MODEL])
        din("b_down", [NEXP, D_MODEL])
    if m == 1:
        din("w_qkv", [D_MODEL, 2048])
        din("g12", [128, 12, 128])
        din("rope", [32, 128, 2, 128])
        din("w_o", [D_MODEL, D_MODEL])
    if m == 2:
        din("s5_are", [2, 128, 64, 64])
        din("s5_aim", [2, 128, 64, 64])
        din("s5_ldt", [2, 128, 64])
        din("s5_bre", [2, 128, 64, 64])
        din("s5_bim", [2, 128, 64, 64])
        din("s5_c1", [2, 128, 64, 16])
        din("s5_c2", [2, 128, 64, 16])
        din("s5_colp", [2, 128, 3, 64])
        din("s5_maskB", [128, 64])
        din("s5_maskJ", [128, 64, 8])
        din("s5_sgn", [128, 2])
        din("s5_d", [128, 8])
        din("w_glu", [D_MODEL, D_MODEL])
        din("w_so", [D_MODEL, D_MODEL])
    if m == 3:
        din("w_in", [D_MODEL, 5, D_MODEL])
        din("lbT", [128, 4, 8])
        din("cmask", [128, HALF])
        din("tri", [64, 2, 64])
        din("hnorm", [64, 8, 128])
        din("w_ho", [D_MODEL, D_MODEL])
    if m == 0:
        din("dftL", [4, 8, 128, 4, 2, 512], BF16)
        din("dftC", [128, 2, 2, 256], BF16)
        din("fnet_w", [D_MODEL, D_MODEL])
        din("fnet_b", [1, D_MODEL])
    D["out"] = nc.dram_tensor("out", [HALF, D_MODEL], F32, kind="ExternalOutput").ap()
    c.modd = nc.dram_tensor("modd", [128, 6 * D_MODEL], F32).ap()
    if "moe" in ph:
        c.x1buf = nc.dram_tensor("x1buf", [HALF, D_MODEL], F32).ap()
    else:
        c.x1buf = nc.dram_tensor("x1buf", [HALF, D_MODEL], F32, kind="ExternalOutput").ap()
    c.h2T = nc.dram_tensor("h2T", [128, 8, HALF], BF16).ap()
    c.gbuf = nc.dram_tensor("gbuf", [16, 128, NEXP], F32).ap()
    ymix = nc.dram_tensor("ymix", [HALF, D_MODEL], F32).ap()
    c.p = Prog(nc)
    setup_consts(c)
    if "mod" in ph:
        phase_mod(c)
    if "mix" in ph:
        if m == 0:
            phase_fnet(c, ymix)
        elif m == 1:
            phase_attn(c, ymix)
        elif m == 2:
            phase_s5(c, ymix)
        elif m == 3:
            phase_hgrn(c, ymix, 3)
    if "post" in ph:
        phase_post(c, ymix)
    if "moe" in ph:
        phase_moe(c)
    c.p.finish()
    c.n_ops = c.p.total_ops
    return nc, c


_PROGS = {}


def get_prog(m):
    if m not in _PROGS:
        _PROGS[m] = build_layer(m)
    return _PROGS[m]


def bf16(a):
    return np.asarray(a, dtype=np.float32).astype(ml_dtypes.bfloat16)


def present(xb, hf):
    return np.ascontiguousarray(xb if hf == 0 else xb[::-1])


def unpresent_idx(hf):
    j = np.arange(HALF)
    return j if hf == 0 else (SEQ - 1 - j)


def fnet_tables(hf):
    j = np.arange(SEQ, dtype=np.int64)
    l_in = j if hf == 0 else SEQ - 1 - j
    l_out = l_in[:HALF]
    ph = (l_in[:, None] * l_out[None, :]) % SEQ
    ang = (2.0 * np.pi / SEQ) * ph.astype(np.float64)
    tab = np.stack([np.cos(ang), np.sin(ang)], axis=1)
    tab = tab.reshape(8, 4, 128, 2, 4, 512).transpose(4, 0, 2, 1, 3, 5)
    return bf16(np.ascontiguousarray(tab))


def fnet_ctab():
    k = np.arange(256, dtype=np.int64)
    ang = (2.0 * np.pi / 256) * ((k[:, None] * k[None, :]) % 256).astype(np.float64)
    t = np.stack([np.cos(ang), -np.sin(ang)], axis=1)
    t = t.reshape(2, 128, 2, 256).transpose(1, 0, 2, 3)
    return bf16(np.ascontiguousarray(t))


def common_inputs(inp, li, b):
    f = np.float32
    d = {}
    d["cT"] = np.ascontiguousarray(inp["c"][b].reshape(8, 128).T).astype(f)
    d["ada_w"] = inp["ada_w"][li]
    d["ada_b"] = inp["ada_b"][li][None, :]
    pl = np.stack([inp["post_ln_g"][li, 0], inp["post_ln_b"][li, 0], inp["post_ln_g"][li, 1], inp["post_ln_b"][li, 1]])
    d["pln"] = np.ascontiguousarray(np.broadcast_to(pl[:, None, :], (4, 128, D_MODEL))).astype(f)
    d["w_r"] = inp["moe_w_router"][li]
    d["b_r"] = inp["moe_b_router"][li][None, :]
    d["w_gu"] = inp["moe_w_gu"][li]
    d["b_guT"] = np.ascontiguousarray(inp["moe_b_gu"][li].reshape(NEXP, 16, 128).transpose(2, 0, 1))
    d["w_down"] = inp["moe_w_down"][li]
    d["b_down"] = inp["moe_b_down"][li]
    return d


def run_layer(li, x, inp):
    m = li % 4
    j = li // 4
    nc, c = get_prog(m)
    in_maps = []
    shared = {}
    for core in range(8):
        b, hf = core // 2, core % 2
        if b not in shared:
            shared[b] = common_inputs(inp, li, b)
        d = dict(shared[b])
        d["xp"] = present(x[b], hf)
        if m == 0:
            d["dftL"] = fnet_tables(hf)
            d["dftC"] = fnet_ctab()
            d["fnet_w"] = inp["fnet_w_out"][j]
            d["fnet_b"] = inp["fnet_b_out"][j][None, :]
        if m == 2:
            d.update(s5_inputs(inp, j, hf))
        if m == 3:
            w = inp["hg_w_in"][j].reshape(D_MODEL, 5, D_MODEL)
            if hf == 1:
                w = w[:, [0, 2, 1, 3, 4], :]
            d["w_in"] = np.ascontiguousarray(w)
            d["lbT"] = np.ascontiguousarray(inp["hg_lb"].reshape(4, 8, 128).transpose(2, 0, 1))
            cmk = np.ones((128, HALF), np.float32)
            cmk[:, ::64] = 0.0
            d["cmask"] = cmk
            sidx = np.arange(64)
            d["tri"] = np.stack([(sidx[:, None] <= sidx[None, :]), (sidx[:, None] >= sidx[None, :])], axis=1).astype(np.float32)
            d["hnorm"] = np.ascontiguousarray(np.broadcast_to(inp["hg_norm"][j].reshape(1, 8, 128), (64, 8, 128))).astype(np.float32)
            d["w_ho"] = inp["hg_w_out"][j]
        if m == 1:
            d["w_qkv"] = inp["attn_w_qkv"][j]
            g = np.concatenate([np.tile(inp["attn_q_norm"][j][None, :], (8, 1)), np.tile(inp["attn_k_norm"][j][None, :], (4, 1))])
            d["g12"] = np.ascontiguousarray(np.broadcast_to(g[None], (128, 12, 128))).astype(np.float32)
            d["rope"] = rope_tables(hf)
            d["w_o"] = inp["attn_w_out"][j]
        in_maps.append({k: v for k, v in d.items() if k in c.D})
    res = run_bass_kernel_spmd(nc, in_maps, core_ids=list(range(8)))
    out = np.empty_like(x)
    key = "out" if "w_gu" in c.D else "x1buf"
    for core in range(8):
        b, hf = core // 2, core % 2
        out[b, unpresent_idx(hf)] = res.results[core][key]
    return out


def phase_attn(c, ymix):
    p, D = c.p, c.D
    SCALE = 128.0 ** -0.5
    with p.scope():
        kT = p.sbuf("kT", [128, 4, SEQ], BF16)
        qT = p.sbuf("qT", [128, 8, HALF], BF16)
        Va = p.sbuf("Va", [128, 32, 4, 129], BF16)
        with p.scope():
            mods = p.sbuf("modsQ", [128, 2, 1024], F32)
            p.dma("sp", mods[:, 0, :], c.modd[:, 0:1024], w=["mods"])
            p.dma("sp", mods[:, 1, :], c.modd[:, 1024:2048], w=["mods"])
            wq = p.sbuf("wqkv", [128, 8, 2048], BF16)
            p.dma("pool", wq[:], D["w_qkv"].rearrange("(c p) n -> p c n", p=128), w=["wq"])
            g12 = p.sbuf("g12", [128, 12, 128], F32)
            p.dma("act", g12[:], D["g12"][:, :, :], w=["g12"])
            p.memset("pool", Va[:, :, :, 128:129], 1.0, w=["Va1"])
            xt = Rot(p, "xtQ", [128, 1024], F32, 2)
            ln = LN(p, "lnQ")
            hb = Rot(p, "hbQ", [128, 1024], BF16, 2)
            hTt = Rot(p, "hTtQ", [128, 8, 128], BF16, 2)
            pT = Rot(p, "pTQ", [128, 4, 128], F32, 3, psum=True)
            pq = Rot(p, "pqQ", [128, 4, 128], F32, 4, psum=True)
            qk = Rot(p, "qkQ", [128, 12, 128], F32, 2)
            sq = p.sbuf("sqQ", [128, 12, 128], F32)
            xr = p.sbuf("xrQ", [128, 12, 128], F32)
            rq = Rot(p, "rqQ", [128, 12, 128], BF16, 2)
            ss = Rot(p, "ssQ", [128, 12], F32, 2)
            rope = Rot(p, "ropeQ", [128, 2, 128], F32, 2)
            for t in range(32):
                own = t < 16
                x, xk = xt.next()
                p.dma("sp", x[:], D["xp"][t * 128:(t + 1) * 128, :], w=[xk])
                h, hk = hb.next()
                ln(x[:], xk, h[:], hk, mods[:, 1, :], mods[:, 0, :], ["mods"])
                hT_, hTk = hTt.next()
                for q in range(2):
                    ps, psk = pT.next()
                    for k in range(4):
                        p.mm(ps[:, k, :], h[:, (q * 4 + k) * 128:(q * 4 + k + 1) * 128], c.identb[:], r=[hk], w=[(psk, k)])
                    p.copy("act" if q == 0 else "dve", hT_[:, q * 4:(q + 1) * 4, :], ps[:],
                           r=[(psk, k) for k in range(4)], w=[(hTk, q)])
                hTk2 = [(hTk, 0), (hTk, 1)]
                qk_, qkk = qk.next()
                for bi in ([0, 1] if own else []) + [2, 3]:
                    pb, pbk = pq.next()
                    pbf = pb[:].rearrange("p h d -> p (h d)")
                    for k in range(8):
                        p.mm(pbf, hT_[:, k, :], wq[:, k, bi * 512:(bi + 1) * 512], start=(k == 0), stop=(k == 7),
                             r=hTk2 + ["wq"], w=[pbk])
                    if bi == 3:
                        p.copy("act", Va[:, t, :, 0:128], pb[:], r=[pbk], w=[("Va", t)])
                    else:
                        p.copy("dve", qk_[:, bi * 4:(bi + 1) * 4, :], pb[:], r=[pbk], w=[(qkk, bi)])
                h0 = 0 if own else 8
                H = 12 - h0
                v = qk_[:, h0:12, :]
                vk = [(qkk, b) for b in (0, 1, 2) if own or b == 2]
                p.tt("pool", sq[:, h0:12, :], v, v, ALU.mult, r=vk, w=["sq"])
                s_, sk = ss.next()
                p.op("dve", (lambda e, s_=s_, h0=h0: e.reduce_sum(out=s_[:, h0:12], in_=sq[:, h0:12, :], axis=AX.X)),
                     r=["sq"], w=[sk])
                p.ts("dve", s_[:, h0:12], s_[:, h0:12], 1.0 / 128.0, ALU.mult, RMS_EPS, ALU.add, r=[sk], w=[sk])
                p.op("act", (lambda e, s_=s_, h0=h0: e.sqrt(out=s_[:, h0:12], in_=s_[:, h0:12])), r=[sk], w=[sk])
                p.op("dve", (lambda e, s_=s_, h0=h0: e.reciprocal(out=s_[:, h0:12], in_=s_[:, h0:12])), r=[sk], w=[sk])
                p.tt("dve", v, v, s_[:, h0:12].unsqueeze(2).to_broadcast([128, H, 128]), ALU.mult, r=vk + [sk], w=vk)
                p.tt("pool", v, v, g12[:, h0:12, :], ALU.mult, r=vk + ["g12"], w=vk)
                rp, rpk = rope.next()
                p.dma("act", rp[:], D["rope"][t], w=[rpk])
                v4 = v.rearrange("p h (b s e) -> p (h b) s e", b=2, s=2, e=32)
                x4 = xr[:, h0:12, :].rearrange("p h (b s e) -> p (h b) s e", b=2, s=2, e=32)
                p.copy("pool", x4[:, :, 0, :], v4[:, :, 1, :], r=vk, w=["xr0"])
                p.copy("pool", x4[:, :, 1, :], v4[:, :, 0, :], r=vk, w=["xr1"])
                cosb = rp[:, 0, :].unsqueeze(1).to_broadcast([128, H, 128])
                sinb = rp[:, 1, :].unsqueeze(1).to_broadcast([128, H, 128])
                p.tt("dve", v, v, cosb, ALU.mult, r=vk + [rpk], w=vk)
                p.tt("pool", xr[:, h0:12, :], xr[:, h0:12, :], sinb, ALU.mult, r=["xr0", "xr1", rpk], w=["xr0", "xr1"])
                r_, rk = rq.next()
                p.tt("dve", r_[:, h0:12, :], v, xr[:, h0:12, :], ALU.add, r=vk + ["xr0", "xr1"], w=[rk])
                for gi, g0 in enumerate(range(h0, 12, 4)):
                    ps, psk = pT.next()
                    for i in range(4):
                        p.mm(ps[:, i, :], r_[:, g0 + i, :], c.identb[:], r=[rk], w=[(psk, i)])
                    if g0 < 8:
                        dst = qT[:, g0:g0 + 4, t * 128:(t + 1) * 128]
                        dk = ("qT", t, g0)
                    else:
                        dst = kT[:, 0:4, t * 128:(t + 1) * 128]
                        dk = ("kT", t)
                    p.copy("act" if gi % 2 == 0 else "dve", dst, ps[:], r=[(psk, i) for i in range(4)], w=[dk])
        Ot = p.sbuf("Otok", [128, 16, 8, 128], BF16)
        with p.scope():
            pS = Rot(p, "pS", [128, 512], F32, 2, psum=True)
            pO = Rot(p, "pO", [128, 512], F32, 4, psum=True)
            PT = Rot(p, "PT", [128, 512], BF16, 3)
            rc = Rot(p, "rc", [128, 1], F32, 4)
            for head in range(8):
                kvh = head // 2
                for qc in range(4):
                    accs = [pO.next() for _ in range(4)]
                    for st in range(32):
                        s_, sk = pS.next()
                        p.mm(s_[:], kT[:, kvh, st * 128:(st + 1) * 128], qT[:, head, qc * 512:(qc + 1) * 512], w=[sk])
                        pt_, ptk = PT.next()
                        p.act(pt_[:], s_[:], ACT.Exp, scale=SCALE, r=[sk], w=[ptk])
                        for qs in range(4):
                            o_, ok = accs[qs]
                            p.mm(o_[:, 0:129], pt_[:, qs * 128:(qs + 1) * 128], Va[:, st, kvh, :],
                                 start=(st == 0), stop=(st == 31), r=[ptk], w=[ok])
                    for qs in range(4):
                        o_, ok = accs[qs]
                        r_, rk = rc.next()
                        p.op("dve", (lambda e, r_=r_, o_=o_: e.reciprocal(out=r_[:], in_=o_[:, 128:129])), r=[ok], w=[rk])
                        p.ts("dve", Ot[:, qc * 4 + qs, head, :], o_[:, 0:128], r_[:, 0:1], ALU.mult, r=[ok, rk],
                             w=[("Ot", qc * 4 + qs, head)])
        with p.scope():
            wo = p.sbuf("woQ", [128, 8, 1024], BF16)
            p.dma("pool", wo[:], D["w_o"].rearrange("(c p) n -> p c n", p=128), w=["wo"])
            pT = Rot(p, "pTo", [128, 4, 128], F32, 2, psum=True)
            pY = Rot(p, "pYo", [128, 512], F32, 2, psum=True)
            oT = Rot(p, "oTo", [128, 8, 128], BF16, 2)
            yo = Rot(p, "yoQ", [128, 1024], F32, 2)
            for qt in range(16):
                o_, otk = oT.next()
                for g in range(2):
                    ps, psk = pT.next()
                    for i in range(4):
                        p.mm(ps[:, i, :], Ot[:, qt, g * 4 + i, :], c.identb[:], w=[(psk, i)])
                    p.copy("act" if g == 0 else "dve", o_[:, g * 4:(g + 1) * 4, :], ps[:],
                           r=[(psk, i) for i in range(4)], w=[(otk, g)])
                y_, yk = yo.next()
                for hh in range(2):
                    py, pyk = pY.next()
                    for hd in range(8):
                        p.mm(py[:], o_[:, hd, :], wo[:, hd, hh * 512:(hh + 1) * 512], start=(hd == 0), stop=(hd == 7),
                             r=[(otk, 0), (otk, 1), "wo"], w=[pyk])
                    p.copy("act" if hh == 0 else "dve", y_[:, hh * 512:(hh + 1) * 512], py[:], r=[pyk], w=[(yk, hh)])
                p.dma("sp", ymix[qt * 128:(qt + 1) * 128, :], y_[:], r=[(yk, 0), (yk, 1)], w=[("ymix", qt)])


def rope_tables(hf):
    j = np.arange(SEQ)
    l = j if hf == 0 else SEQ - 1 - j
    row = (l // 64).astype(np.float32)
    col = (l % 64).astype(np.float32)
    inv = (np.float32(10000.0) ** (-np.arange(0, 64, 2, dtype=np.float32) / np.float32(64))).astype(np.float32)
    ar = (row[:, None] * inv[None, :]).astype(np.float32)
    ac = (col[:, None] * inv[None, :]).astype(np.float32)
    cr, sr, cc, sc = np.cos(ar), np.sin(ar), np.cos(ac), np.sin(ac)
    cos = np.concatenate([cr, cr, cc, cc], axis=1)
    sin = np.concatenate([-sr, sr, -sc, sc], axis=1)
    t = np.stack([cos, sin], axis=1).astype(np.float32)
    return np.ascontiguousarray(t.reshape(32, 128, 2, 128))


def phase_hgrn(c, ymix, li):
    p, D = c.p, c.D
    CH = 64
    with p.scope():
        hTd = c.nc.dram_tensor("hTdH" + getattr(c, "lsuffix", ""), [128, 8, SEQ], BF16).ap()
        oTall = p.sbuf("oTall", [128, 8, HALF], BF16)
        lbt = p.sbuf("lbt", [128, 4, 8], F32)
        lb = p.sbuf("lb", [128, 8], F32)
        oml = p.sbuf("oml", [128, 8], F32)
        noml = p.sbuf("noml", [128, 8], F32)
        den = p.sbuf("lbden", [128, 8], F32)
        cm = p.sbuf("cmaskH", [128, HALF], F32)
        tri = p.sbuf("triH", [64, 2, 64], F32)
        hn = p.sbuf("hnH", [64, 8, 128], F32)
        p.dma("sp", lbt[:], D["lbT"][:, :, :], w=["lbt"])
        p.dma("sp", cm[:], D["cmask"][:, :], w=["cm"])
        p.dma("sp", tri[:], D["tri"][:, :, :], w=["tri"])
        p.dma("sp", hn[:], D["hnorm"][:, :, :], w=["hn"])
        p.act(lbt[:], lbt[:], ACT.Exp, r=["lbt"], w=["lbt"])
        p.copy("dve", den[:], lbt[:, 0, :], r=["lbt"], w=["den"])
        for j in range(1, 4):
            p.tt("dve", den[:], den[:], lbt[:, j, :], ALU.add, r=["den", "lbt"], w=["den"])
        p.memset("dve", lb[:], 0.0, w=["lb"])
        for j in range(1, li + 1):
            p.tt("dve", lb[:], lb[:], lbt[:, j, :], ALU.add, r=["lb", "lbt"], w=["lb"])
        p.op("dve", (lambda e: e.reciprocal(out=den[:], in_=den[:])), r=["den"], w=["den"])
        p.tt("dve", lb[:], lb[:], den[:], ALU.mult, r=["lb", "den"], w=["lb"])
        p.ts("dve", oml[:], lb[:], -1.0, ALU.mult, 1.0, ALU.add, r=["lb"], w=["oml"])
        p.ts("dve", noml[:], oml[:], -1.0, ALU.mult, r=["oml"], w=["noml"])
        with p.scope():
            mods = p.sbuf("modsH", [128, 2, 1024], F32)
            p.dma("sp", mods[:, 0, :], c.modd[:, 0:1024], w=["mods"])
            p.dma("sp", mods[:, 1, :], c.modd[:, 1024:2048], w=["mods"])
            xt = Rot(p, "xtH", [128, 1024], F32, 2)
            ln = LN(p, "lnH")
            hb = Rot(p, "hbH", [128, 1024], BF16, 2)
            pT = Rot(p, "pTH", [128, 4, 128], F32, 4, psum=True)
            hTt = Rot(p, "hTtH", [128, 8, 128], BF16, 2)
            for t in range(32):
                x, xk = xt.next()
                p.dma("sp" if t % 2 == 0 else "act", x[:], D["xp"][t * 128:(t + 1) * 128, :], w=[xk])
                h, hk = hb.next()
                ln(x[:], xk, h[:], hk, mods[:, 1, :], mods[:, 0, :], ["mods"])
                hT_, hTk = hTt.next()
                for q in range(2):
                    ps, psk = pT.next()
                    for k in range(4):
                        p.mm(ps[:, k, :], h[:, (q * 4 + k) * 128:(q * 4 + k + 1) * 128], c.identb[:], r=[hk], w=[(psk, k)])
                    p.copy("act" if q == 0 else "dve", hT_[:, q * 4:(q + 1) * 4, :], ps[:],
                           r=[(psk, k) for k in range(4)], w=[(hTk, q)])
                p.dma("sp", hTd[:, :, t * 128:(t + 1) * 128], hT_[:], r=[(hTk, 0), (hTk, 1)], w=[("hTd", t)])
        p.flush()
        with p.scope():
            wh = Rot(p, "whH", [128, 8, 5, 128], BF16, 1)
            hs = p.sbuf("hsH", [128, 8, HALF], BF16)
            qf = p.sbuf("qfH", [128, HALF], F32)
            t1 = p.sbuf("t1H", [128, HALF], F32)
            kf = p.sbuf("kfH", [128, HALF], F32)
            bb = p.sbuf("bbH", [128, HALF], F32)
            kl = p.sbuf("klH", [128, HALF], BF16)
            qd = p.sbuf("qdH", [128, HALF], BF16)
            kk = p.sbuf("kkH", [128, HALF], BF16)
            dec = p.sbuf("decH", [128, 32], F32)
            vtok = p.sbuf("vtokH", [64, 32, 128], BF16)
            oA = p.sbuf("oAH", [64, 32, 128], F32)
            sgt = p.sbuf("sgtH", [64, 32, 128], BF16)
            og = p.sbuf("ogH", [64, 32, 128], BF16)
            ssn = p.sbuf("ssnH", [64, 32], F32)
            S = p.sbuf("SH", [128, 128], F32)
            Sb = p.sbuf("SbH", [128, 128], BF16)
            klt = Rot(p, "kltH", [64, 128], BF16, 2)
            attT = Rot(p, "attTH", [64, 64], BF16, 2)
            pbig = Rot(p, "pbigH", [128, 512], F32, 2, psum=True)
            pkv = Rot(p, "pkvH", [128, 512], F32, 2, psum=True)
            pa = Rot(p, "paH", [128, 512], F32, 2, psum=True)
            po = Rot(p, "poH", [128, 512], F32, 2, psum=True)
            for h in range(8):
                w_, wk = wh.next()
                fmap = [0, 2, 1, 3, 4] if getattr(c, "vhf", 0) else [0, 1, 2, 3, 4]
                for f_ in range(5):
                    p.dma("pool", w_[:, :, f_, :], D["w_in"][:, fmap[f_], h * 128:(h + 1) * 128].rearrange("(c p) n -> p c n", p=128), w=[wk])

                def proj_fm(fi, dst, func, dk_):
                    for blk in range(4):
                        ps, psk = pbig.next()
                        for k in range(8):
                            p.mm(ps[:], w_[:, k, fi, :], hs[:, k, blk * 512:(blk + 1) * 512],
                                 start=(k == 0), stop=(k == 7), r=[wk, "hs"], w=[psk])
                        p.act(dst[:, blk * 512:(blk + 1) * 512], ps[:], func, r=[psk], w=[(dk_, blk)])
                    return [(dk_, b) for b in range(4)]

                def proj_tm(fi, nchunks, dst, dk_, func):
                    for n0 in range(0, nchunks, 4):
                        ps, psk = pbig.next()
                        for i in range(4):
                            n = n0 + i
                            for k in range(8):
                                p.mm(ps[0:64, i * 128:(i + 1) * 128], hs[:, k, n * CH:(n + 1) * CH], w_[:, k, fi, :],
                                     start=(k == 0), stop=(k == 7), r=[wk, "hs"], w=[(psk, i)])
                        src = ps[0:64, :].rearrange("p (i d) -> p i d", i=4)
                        p.act(dst[:, n0:n0 + 4, :], src, func, r=[(psk, i) for i in range(4)], w=[(dk_, n0)])

                qk_ = [("qf", b_) for b_ in range(4)]
                vkeys = [("vt", n0) for n0 in range(0, 32, 4)]
                p.memset("dve", S[:], 0.0, w=["S"])
                p.memset("pool", Sb[:], 0.0, w=["Sb"])
                for seg, (fi, tok0, outputs, rev) in enumerate([(1, 0, True, False), (2, HALF, False, True), (2, 0, True, True)]):
                    if seg == 1:
                        p.memset("dve", S[:], 0.0, w=["S"])
                        p.memset("pool", Sb[:], 0.0, w=["Sb"])
                    p.dma("sp", hs[:], hTd[:, :, tok0:tok0 + HALF], w=["hs"])
                    if seg == 0:
                        proj_fm(0, qf, ACT.Silu, "qf")
                        proj_tm(4, 32, sgt, "sgt", ACT.Silu)
                    proj_tm(3, 32, vtok, "vt", ACT.Copy)
                    tk = proj_fm(fi, t1, ACT.Sigmoid, "t1")
                    p.ts("pool", kf[:], t1[:], noml[:, h:h + 1], ALU.mult, oml[:, h:h + 1], ALU.add, r=tk + ["noml", "oml"], w=["kf"])
                    p.ts("dve", t1[:], t1[:], oml[:, h:h + 1], ALU.mult, lb[:, h:h + 1], ALU.add, r=tk + ["oml", "lb"], w=tk)
                    p.act(t1[:], t1[:], ACT.Ln, r=tk, w=tk)
                    if rev:
                        p.op("dve", (lambda e: e.tensor_tensor_scan(out=bb[:, ::-1], data0=cm[:], data1=t1[:, ::-1], initial=0.0,
                                                                     op0=ALU.mult, op1=ALU.add)), r=tk + ["cm"], w=["bb"])
                    else:
                        p.op("dve", (lambda e: e.tensor_tensor_scan(out=bb[:], data0=cm[:], data1=t1[:], initial=0.0,
                                                                     op0=ALU.mult, op1=ALU.add)), r=tk + ["cm"], w=["bb"])
                    b3 = bb[:].rearrange("p (n c) -> p n c", c=CH)
                    bl = b3[:, :, 0:1] if rev else b3[:, :, CH - 1:CH]
                    t3 = t1[:].rearrange("p (n c) -> p n c", c=CH)
                    p.tt("pool", t3, bl.to_broadcast([128, 32, CH]), b3, ALU.subtract, r=["bb"] + tk, w=tk)
                    p.act(t1[:], t1[:], ACT.Exp, r=tk, w=tk)
                    p.tt("dve", kl[:], kf[:], t1[:], ALU.mult, r=["kf"] + tk, w=["kl"])
                    p.act(dec[:].unsqueeze(2), bl, ACT.Exp, r=["bb"], w=["dec"])
                    if outputs:
                        p.act(t1[:], bb[:], ACT.Exp, r=["bb", "kl"] + tk, w=tk)
                        p.tt("pool", qd[:], qf[:], t1[:], ALU.mult, r=qk_ + tk, w=["qd"])
                        p.act(t1[:], bb[:], ACT.Exp, scale=-1.0, r=["bb", "qd"] + tk, w=tk)
                        p.tt("dve", kk[:], kf[:], t1[:], ALU.mult, r=["kf"] + tk, w=["kk"])
                    order = range(31, -1, -1) if rev else range(32)
                    vbase = 0
                    for n in order:
                        cs = slice(n * CH, (n + 1) * CH)
                        ps, psk = pa.next()
                        p.mm(ps[0:64, 0:128], kl[:, cs], c.identb[:], r=["kl"], w=[psk])
                        kt_, ktk = klt.next()
                        p.copy("act", kt_[:], ps[0:64, 0:128], r=[psk], w=[ktk])
                        pv, pvk = pkv.next()
                        p.mm(pv[:, 0:128], kt_[:], vtok[:, vbase + n, :], r=[ktk, vkeys[n // 4]], w=[pvk])
                        if outputs:
                            ps2, ps2k = pa.next()
                            p.mm(ps2[0:64, 0:64], kk[:, cs], qd[:, cs], r=["kk", "qd"], w=[ps2k])
                            at_, atk = attT.next()
                            p.tt("dve", at_[:], ps2[0:64, 0:64], tri[:, 1 if rev else 0, :], ALU.mult, r=[ps2k, "tri"], w=[atk])
                            po_, pok = po.next()
                            p.mm(po_[0:64, 0:128], at_[:], vtok[:, vbase + n, :], start=True, stop=False,
                                 r=[atk, vkeys[n // 4]], w=[pok])
                            p.mm(po_[0:64, 0:128], qd[:, cs], Sb[:], start=False, stop=True, r=["qd", "Sb"], w=[pok])
                            if seg == 0:
                                p.copy("act", oA[:, n, :], po_[0:64, 0:128], r=[pok], w=[("oA", n)])
                            else:
                                p.tt("dve", oA[:, n, :], oA[:, n, :], po_[0:64, 0:128], ALU.add, r=[pok, ("oA", n)], w=[("oA", n)])
                        p.stt("dve", S[:], S[:], dec[:, n:n + 1], pv[:, 0:128], ALU.mult, ALU.add, r=["S", "dec", pvk], w=["S"])
                        p.copy("pool", Sb[:], S[:], r=["S"], w=["Sb"])
                oAk = [("oA", n) for n in range(32)]
                p.tt("pool", og[:], oA[:], oA[:], ALU.mult, r=oAk, w=["og"])
                p.op("dve", (lambda e: e.reduce_sum(out=ssn[:], in_=og[:], axis=AX.X)), r=["og"], w=["ssn"])
                p.ts("dve", ssn[:], ssn[:], 1.0 / 128.0, ALU.mult, RMS_EPS, ALU.add, r=["ssn"], w=["ssn"])
                p.op("act", (lambda e: e.sqrt(out=ssn[:], in_=ssn[:])), r=["ssn"], w=["ssn"])
                p.op("dve", (lambda e: e.reciprocal(out=ssn[:], in_=ssn[:])), r=["ssn"], w=["ssn"])
                p.tt("dve", oA[:], oA[:], ssn[:].unsqueeze(2).to_broadcast([64, 32, 128]), ALU.mult, r=oAk + ["ssn"], w=oAk)
                p.tt("pool", oA[:], oA[:], hn[:, h, :].unsqueeze(1).to_broadcast([64, 32, 128]), ALU.mult, r=oAk + ["hn"], w=oAk)
                p.tt("dve", og[:], oA[:], sgt[:], ALU.mult, r=oAk + [("sgt", n0) for n0 in range(0, 32, 4)], w=["og"])
                for n0 in range(0, 32, 8):
                    ps, psk = pbig.next()
                    for i in range(8):
                        p.mm(ps[:, i * 64:(i + 1) * 64], og[:, n0 + i, :], c.identb[0:64, 0:64], r=["og"], w=[(psk, i)])
                    p.copy("act", oTall[:, h, n0 * 64:(n0 + 8) * 64], ps[:], r=[(psk, i) for i in range(8)], w=[("oTall", h, n0)])
        with p.scope():
            wo = p.sbuf("woH", [128, 8, 1024], BF16)
            p.dma("pool", wo[:], D["w_ho"].rearrange("(c p) n -> p c n", p=128), w=["wo"])
            pY = Rot(p, "pYH", [128, 512], F32, 2, psum=True)
            yo = Rot(p, "yoH", [128, 1024], F32, 2)
            for qt in range(16):
                y_, yk = yo.next()
                for hh in range(2):
                    py, pyk = pY.next()
                    for hd in range(8):
                        p.mm(py[:], oTall[:, hd, qt * 128:(qt + 1) * 128], wo[:, hd, hh * 512:(hh + 1) * 512],
                             start=(hd == 0), stop=(hd == 7), r=["wo"], w=[pyk])
                    p.copy("act" if hh == 0 else "dve", y_[:, hh * 512:(hh + 1) * 512], py[:], r=[pyk], w=[(yk, hh)])
                p.dma("sp", ymix[qt * 128:(qt + 1) * 128, :], y_[:], r=[(yk, 0), (yk, 1)], w=[("ymix", qt)])


def _sincos(p, th, sn, cs, tmp, t2, keys_r, kout):
    PI = math.pi
    for which, dst in ((0, sn), (1, cs)):
        if which == 0:
            p.copy("dve", t2, th, r=keys_r, w=[kout + "_t2"])
        else:
            p.ts("dve", t2, th, PI / 2, ALU.add, r=keys_r, w=[kout + "_t2"])
        p.copy("dve", dst, t2, r=[kout + "_t2"], w=[kout + str(which)])
        for i in range(1, 9):
            p.ts("dve", tmp, t2, (2 * i - 1) * PI, ALU.is_gt, 2 * PI, ALU.mult, r=[kout + "_t2"], w=[kout + "_tmp"])
            p.tt("dve", dst, dst, tmp, ALU.subtract, r=[kout + str(which), kout + "_tmp"], w=[kout + str(which)])
        p.act(dst, dst, ACT.Sin, r=[kout + str(which)], w=[kout + str(which)])


def phase_s5(c, ymix):
    p, D = c.p, c.D
    with p.scope():
        hTd = c.nc.dram_tensor("hTdS" + getattr(c, "lsuffix", ""), [128, 8, SEQ], BF16).ap()
        ygT = p.sbuf("ygT", [128, 8, HALF], BF16)
        with p.scope():
            mods = p.sbuf("modsS", [128, 2, 1024], F32)
            p.dma("sp", mods[:, 0, :], c.modd[:, 0:1024], w=["mods"])
            p.dma("sp", mods[:, 1, :], c.modd[:, 1024:2048], w=["mods"])
            xt = Rot(p, "xtS", [128, 1024], F32, 2)
            ln = LN(p, "lnS")
            hb = Rot(p, "hbS", [128, 1024], BF16, 2)
            pT = Rot(p, "pTS", [128, 4, 128], F32, 4, psum=True)
            hTt = Rot(p, "hTtS", [128, 8, 128], BF16, 2)
            for t in range(32):
                x, xk = xt.next()
                p.dma("sp" if t % 2 == 0 else "act", x[:], D["xp"][t * 128:(t + 1) * 128, :], w=[xk])
                h, hk = hb.next()
                ln(x[:], xk, h[:], hk, mods[:, 1, :], mods[:, 0, :], ["mods"])
                hT_, hTk = hTt.next()
                for q in range(2):
                    ps, psk = pT.next()
                    for k in range(4):
                        p.mm(ps[:, k, :], h[:, (q * 4 + k) * 128:(q * 4 + k + 1) * 128], c.identb[:], r=[hk], w=[(psk, k)])
                    p.copy("act" if q == 0 else "dve", hT_[:, q * 4:(q + 1) * 4, :], ps[:],
                           r=[(psk, k) for k in range(4)], w=[(hTk, q)])
                p.dma("sp", hTd[:, :, t * 128:(t + 1) * 128], hT_[:], r=[(hTk, 0), (hTk, 1)], w=[("hTd", t)])
        with p.scope():
            colp = p.sbuf("colp", [128, 2, 3, 64], F32)
            vsw = getattr(c, "vhf", 0)
            for dd_ in range(2):
                p.dma("sp", colp[:, dd_, :, :], D["s5_colp"][dd_ ^ vsw], w=["colp"])
            rc = p.sbuf("rcS", [128, 2, 64], F32)
            thc = p.sbuf("thcS", [128, 2, 64], F32)
            DC = p.sbuf("DCS", [128, 2, 12, 64], F32)
            DS = p.sbuf("DSS", [128, 2, 12, 64], F32)
            NDS = p.sbuf("NDSS", [128, 2, 12, 64], F32)
            c1 = p.sbuf("c1S", [128, 2, 64], F32)
            c2 = p.sbuf("c2S", [128, 2, 64], F32)
            c3 = p.sbuf("c3S", [128, 2, 64], F32)
            c4 = p.sbuf("c4S", [128, 2, 64], F32)
            c5 = p.sbuf("c5S", [128, 2, 64], F32)
            c6 = p.sbuf("c6S", [128, 2, 64], F32)
            p.act(c1[:], colp[:, :, 2, :], ACT.Exp, r=["colp"], w=["c1"])
            p.tt("dve", rc[:], colp[:, :, 0, :], c1[:], ALU.mult, r=["colp", "c1"], w=["rc"])
            p.act(rc[:], rc[:], ACT.Exp, r=["rc"], w=["rc"])
            p.tt("dve", thc[:], colp[:, :, 1, :], c1[:], ALU.mult, r=["colp", "c1"], w=["thc"])
            _sincos(p, thc[:], c2[:], c3[:], c5[:], c6[:], ["thc"], "scC")
            p.copy("dve", DS[:, :, 0, :], c2[:], r=["scC0"], w=[("DS", 0)])
            p.copy("dve", DC[:, :, 0, :], c3[:], r=["scC1"], w=[("DC", 0)])
            for k in range(1, 12):
                cp_, sp_ = DC[:, :, k - 1, :], DS[:, :, k - 1, :]
                p.tt("dve", c1[:], cp_, cp_, ALU.mult, r=[("DC", k - 1)], w=["c1"])
                p.tt("dve", c4[:], sp_, sp_, ALU.mult, r=[("DS", k - 1)], w=["c4"])
                p.tt("dve", DC[:, :, k, :], c1[:], c4[:], ALU.subtract, r=["c1", "c4"], w=[("DC", k)])
                p.stt("dve", DS[:, :, k, :], cp_, 2.0, sp_, ALU.mult, ALU.mult, r=[("DC", k - 1), ("DS", k - 1)], w=[("DS", k)])
            p.ts("dve", NDS[:], DS[:], -1.0, ALU.mult, r=[("DS", k) for k in range(12)], w=["NDS"])
            cst = [("DC", k) for k in range(12)] + [("DS", k) for k in range(12)] + ["NDS", "rc"]
            mB = p.sbuf("mBS", [128, 64], F32)
            mJ = p.sbuf("mJS", [128, 64, 8], F32)
            sgn = p.sbuf("sgnS", [128, 2], F32)
            dcol = p.sbuf("dcolS", [128, 8], F32)
            p.dma("act", mB[:], D["s5_maskB"][:, :], w=["mB"])
            p.dma("act", mJ[:], D["s5_maskJ"][:, :, :], w=["mJ"])
            p.dma("act", sgn[:], D["s5_sgn"][:, :], w=["sgn"])
            p.dma("act", dcol[:], D["s5_d"][:, :], w=["dcol"])
            Ct = p.sbuf("CtS", [128, SEQ], F32)
            St = p.sbuf("StS", [128, SEQ], F32)
            wt = p.sbuf("wtS", [128, SEQ], F32)
            zz = p.sbuf("zzS", [128, SEQ], F32)
            tk_ = p.sbuf("tkS", [128, HALF], F32)
            hTc = Rot(p, "hTcS", [128, SEQ], BF16, 1)
            tw = Rot(p, "twS", [128, 512], F32, 2)
            P1 = Rot(p, "P1S", [128, 512], BF16, 2)
            P2 = Rot(p, "P2S", [128, 512], BF16, 2)
            yv = Rot(p, "yvS", [128, 512], F32, 2)
            R = {n: p.sbuf("R%sS" % n, [128, 8, 64], F32) for n in
                 ("are", "aim", "bre", "bim", "ar", "th", "sn", "cs", "t1", "t2", "x", "y", "kr", "ki", "s1", "s2")}
            ldt = p.sbuf("ldtS", [128, 8], F32)
            c1s = p.sbuf("c1sS", [128, 8, 16], F32)
            c2s = p.sbuf("c2sS", [128, 8, 16], F32)
            tmpC = p.sbuf("tmpCS", [128, 8, 8, 16], F32)
            PRM = {(dd, n): p.sbuf("prm%s%d" % (n, dd), [128, 8, 128], BF16) for dd in range(2) for n in ("B", "Bs", "C1", "C2")}
            pbu = Rot(p, "pbuS", [128, 512], F32, 2, psum=True)
            pbs = Rot(p, "pbsS", [128, 512], F32, 2, psum=True)
            yacc = [p.psum("yaccS%d" % i, [128, 512], F32) for i in range(4)]
            for cc in range(8):
                hc, hck = hTc.next()
                p.dma("sp", hc[:], hTd[:, cc, :], w=[hck])
                gs = slice(cc * 8, (cc + 1) * 8)
                for dd in range(2):
                    for n, src in (("are", "s5_are"), ("aim", "s5_aim"), ("bre", "s5_bre"), ("bim", "s5_bim")):
                        p.dma("act", R[n][:], D[src][dd ^ vsw, :, gs, :], w=["R" + n])
                    p.dma("act", ldt[:], D["s5_ldt"][dd ^ vsw, :, gs], w=["ldt"])
                    p.dma("act", c1s[:], D["s5_c1"][dd ^ vsw, :, gs, :], w=["c1s"])
                    p.dma("act", c2s[:], D["s5_c2"][dd ^ vsw, :, gs, :], w=["c2s"])
                    p.act(ldt[:], ldt[:], ACT.Exp, r=["ldt"], w=["ldt"])
                    dtb = ldt[:].unsqueeze(2).to_broadcast([128, 8, 64])
                    p.tt("dve", R["ar"][:], R["are"][:], dtb, ALU.mult, r=["Rare", "ldt"], w=["Rar"])
                    p.tt("dve", R["th"][:], R["aim"][:], dtb, ALU.mult, r=["Raim", "ldt"], w=["Rth"])
                    p.act(R["ar"][:], R["ar"][:], ACT.Exp, r=["Rar"], w=["Rar"])
                    _sincos(p, R["th"][:], R["sn"][:], R["cs"][:], R["s1"][:], R["s2"][:], ["Rth"], "scR")
                    p.tt("dve", R["x"][:], R["ar"][:], R["cs"][:], ALU.mult, r=["Rar", "scR1"], w=["Rx"])
                    p.ts("dve", R["x"][:], R["x"][:], -1.0, ALU.add, r=["Rx"], w=["Rx"])
                    p.tt("dve", R["y"][:], R["ar"][:], R["sn"][:], ALU.mult, r=["Rar", "scR0"], w=["Ry"])
                    p.tt("dve", R["t1"][:], R["are"][:], R["are"][:], ALU.mult, r=["Rare"], w=["Rt1"])
                    p.tt("dve", R["t2"][:], R["aim"][:], R["aim"][:], ALU.mult, r=["Raim"], w=["Rt2"])
                    p.tt("dve", R["t1"][:], R["t1"][:], R["t2"][:], ALU.add, r=["Rt1", "Rt2"], w=["Rt1"])
                    p.op("dve", (lambda e: e.reciprocal(out=R["t1"][:], in_=R["t1"][:])), r=["Rt1"], w=["Rt1"])
                    p.tt("dve", R["kr"][:], R["x"][:], R["are"][:], ALU.mult, r=["Rx", "Rare"], w=["Rkr"])
                    p.tt("dve", R["t2"][:], R["y"][:], R["aim"][:], ALU.mult, r=["Ry", "Raim"], w=["Rt2"])
                    p.tt("dve", R["kr"][:], R["kr"][:], R["t2"][:], ALU.add, r=["Rkr", "Rt2"], w=["Rkr"])
                    p.tt("dve", R["kr"][:], R["kr"][:], R["t1"][:], ALU.mult, r=["Rkr", "Rt1"], w=["Rkr"])
                    p.tt("dve", R["ki"][:], R["y"][:], R["are"][:], ALU.mult, r=["Ry", "Rare"], w=["Rki"])
                    p.tt("dve", R["t2"][:], R["x"][:], R["aim"][:], ALU.mult, r=["Rx", "Raim"], w=["Rt2"])
                    p.tt("dve", R["ki"][:], R["ki"][:], R["t2"][:], ALU.subtract, r=["Rki", "Rt2"], w=["Rki"])
                    p.tt("dve", R["ki"][:], R["ki"][:], R["t1"][:], ALU.mult, r=["Rki", "Rt1"], w=["Rki"])
                    p.tt("dve", R["x"][:], R["kr"][:], R["bre"][:], ALU.mult, r=["Rkr", "Rbre"], w=["Rx"])
                    p.tt("dve", R["t2"][:], R["ki"][:], R["bim"][:], ALU.mult, r=["Rki", "Rbim"], w=["Rt2"])
                    p.tt("dve", R["x"][:], R["x"][:], R["t2"][:], ALU.subtract, r=["Rx", "Rt2"], w=["Rx"])
                    p.tt("dve", R["y"][:], R["kr"][:], R["bim"][:], ALU.mult, r=["Rkr", "Rbim"], w=["Ry"])
                    p.tt("dve", R["t2"][:], R["ki"][:], R["bre"][:], ALU.mult, r=["Rki", "Rbre"], w=["Rt2"])
                    p.tt("dve", R["y"][:], R["y"][:], R["t2"][:], ALU.add, r=["Ry", "Rt2"], w=["Ry"])
                    mBb = mB[:, gs].unsqueeze(2).to_broadcast([128, 8, 64])
                    Bp, Bs = PRM[(dd, "B")], PRM[(dd, "Bs")]
                    kB, kBs = ("prm", dd, "B"), ("prm", dd, "Bs")
                    p.tt("dve", Bp[:, :, 0:64], R["x"][:], mBb, ALU.mult, r=["Rx", "mB"], w=[(kB, 0)])
                    p.tt("dve", Bp[:, :, 64:128], R["y"][:], mBb, ALU.mult, r=["Ry", "mB"], w=[(kB, 1)])
                    p.tt("dve", Bs[:, :, 0:64], R["y"][:], mBb, ALU.mult, r=["Ry", "mB"], w=[(kBs, 0)])
                    p.stt("dve", Bs[:, :, 64:128], R["x"][:], -1.0, mBb, ALU.mult, ALU.mult, r=["Rx", "mB"], w=[(kBs, 1)])
                    mJb = mJ[:, gs, :].unsqueeze(3).to_broadcast([128, 8, 8, 16])
                    for n, cs_, col in (("C1", c1s, 0), ("C2", c2s, 1)):
                        p.tt("dve", tmpC[:], cs_[:].unsqueeze(2).to_broadcast([128, 8, 8, 16]), mJb, ALU.mult,
                             r=["c1s", "c2s", "mJ"], w=["tmpC"])
                        p.ts("dve", PRM[(dd, n)][:].rearrange("p g (j c) -> p g j c", j=8), tmpC[:], sgn[:, col:col + 1], ALU.mult,
                             r=["tmpC", "sgn"], w=[("prm", dd, n)])
                first = True
                for dd in range(2):
                    T = HALF if dd == 0 else SEQ
                    nst = 11 if dd == 0 else 12
                    for g8 in range(8):
                        g = cc * 8 + g8
                        p.memset("pool", Ct[:, 0:1], 1.0, w=["Ct", ("Ct", 0)])
                        p.memset("pool", St[:, 0:1], 0.0, w=["St", ("St", 0)])
                        for k in range(nst):
                            m = 1 << k
                            dc, ds, nds = DC[:, dd, k, g:g + 1], DS[:, dd, k, g:g + 1], NDS[:, dd, k, g:g + 1]
                            ctlo = [("Ct", q) for q in range(k + 1)]
                            stlo = [("St", q) for q in range(k + 1)]
                            p.ts("dve", tk_[:, 0:m], Ct[:, 0:m], dc, ALU.mult, r=ctlo + cst, w=["tk"])
                            p.ts("dve", zz[:, 0:m], St[:, 0:m], dc, ALU.mult, r=stlo, w=["zz"])
                            last_ = (k == nst - 1)
                            p.stt("dve", Ct[:, m:2 * m], St[:, 0:m], nds, tk_[:, 0:m], ALU.mult, ALU.add, r=stlo + ["tk"],
                                  w=[("Ct", k + 1)] + (["Ct"] if last_ else []))
                            p.stt("dve", St[:, m:2 * m], Ct[:, 0:m], ds, zz[:, 0:m], ALU.mult, ALU.add, r=ctlo + ["zz"],
                                  w=[("St", k + 1)] + (["St"] if last_ else []))
                        kB, kBs = ("prm", dd, "B"), ("prm", dd, "Bs")
                        for blk in range(T // 512):
                            j0 = blk * 512
                            pb, pbk = pbu.next()
                            ps_, psk = pbs.next()
                            p.mm(pb[:], PRM[(dd, "B")][:, g8, :], hc[:, j0:j0 + 512], r=[(kB, 0), (kB, 1), hck], w=[pbk])
                            p.mm(ps_[:], PRM[(dd, "Bs")][:, g8, :], hc[:, j0:j0 + 512], r=[(kBs, 0), (kBs, 1), hck], w=[psk])
                            if dd == 0:
                                tsl = slice(j0, j0 + 512)
                            else:
                                a = SEQ - 1 - j0
                                b = a - 512
                                tsl = slice(a, b if b >= 0 else None, -1)
                            t_, tk2 = tw.next()
                            p.tt("dve", t_[:], Ct[:, tsl], pb[:], ALU.mult, r=["Ct", pbk], w=[tk2])
                            p.tt("dve", wt[:, tsl], St[:, tsl], ps_[:], ALU.mult, r=["St", psk], w=[("wt", blk)])
                            p.tt("pool", wt[:, tsl], wt[:, tsl], t_[:], ALU.add, r=[("wt", blk), tk2], w=[("wt", blk)])
                        wk_ = [("wt", blk) for blk in range(T // 512)]
                        p.op("dve", (lambda e, T=T, dd=dd, g=g: e.tensor_tensor_scan(
                            out=zz[:, 0:T], data0=rc[:, dd, g:g + 1].to_broadcast([128, T]), data1=wt[:, 0:T],
                            initial=0.0, op0=ALU.mult, op1=ALU.add)), r=wk_ + ["rc"], w=["zz"])
                        for blk in range(4):
                            j0 = blk * 512
                            if dd == 0:
                                tsl = slice(j0, j0 + 512)
                            else:
                                a = SEQ - 1 - j0
                                tsl = slice(a, a - 512, -1)
                            p1, p1k = P1.next()
                            p2, p2k = P2.next()
                            p.tt("dve", p1[:], Ct[:, tsl], zz[:, tsl], ALU.mult, r=["Ct", "zz"], w=[p1k])
                            p.tt("pool", p2[:], St[:, tsl], zz[:, tsl], ALU.mult, r=["St", "zz"], w=[p2k])
                            last = (dd == 1 and g8 == 7)
                            p.mm(yacc[blk][:], PRM[(dd, "C1")][:, g8, :], p1[:], start=first, stop=False,
                                 r=[("prm", dd, "C1"), p1k], w=[("yacc", blk)])
                            p.mm(yacc[blk][:], PRM[(dd, "C2")][:, g8, :], p2[:], start=False, stop=last,
                                 r=[("prm", dd, "C2"), p2k], w=[("yacc", blk)])
                        first = False
                for blk in range(4):
                    y_, yk = yv.next()
                    p.stt("dve", y_[:], hc[:, blk * 512:(blk + 1) * 512], dcol[:, cc:cc + 1], yacc[blk][:], ALU.mult, ALU.add,
                          r=[hck, "dcol", ("yacc", blk)], w=[yk])
                    p.act(ygT[:, cc, blk * 512:(blk + 1) * 512], y_[:], ACT.Gelu_apprx_tanh, r=[yk], w=[("ygT", cc, blk)])
        with p.scope():
            wg = p.sbuf("wgS", [128, 8, 1024], BF16)
            wo = p.sbuf("woS", [128, 8, 1024], BF16)
            p.dma("pool", wg[:], D["w_glu"].rearrange("(c p) n -> p c n", p=128), w=["wg"])
            p.dma("pool", wo[:], D["w_so"].rearrange("(c p) n -> p c n", p=128), w=["wo"])
            y2T = p.sbuf("y2TS", [128, 8, HALF], BF16)
            pz = Rot(p, "pzS", [128, 512], F32, 2, psum=True)
            sg = Rot(p, "sgS", [128, 512], F32, 2)
            for oc in range(8):
                for blk in range(4):
                    ps, psk = pz.next()
                    for k in range(8):
                        p.mm(ps[:], wg[:, k, oc * 128:(oc + 1) * 128], ygT[:, k, blk * 512:(blk + 1) * 512],
                             start=(k == 0), stop=(k == 7), r=["wg"], w=[psk])
                    s_, sk = sg.next()
                    p.act(s_[:], ps[:], ACT.Sigmoid, r=[psk], w=[sk])
                    p.tt("dve", y2T[:, oc, blk * 512:(blk + 1) * 512], ygT[:, oc, blk * 512:(blk + 1) * 512], s_[:], ALU.mult,
                         r=[sk], w=[("y2T", oc, blk)])
            pY = Rot(p, "pYS", [128, 512], F32, 2, psum=True)
            yo = Rot(p, "yoS", [128, 1024], F32, 2)
            for qt in range(16):
                y_, yk = yo.next()
                for hh in range(2):
                    py, pyk = pY.next()
                    for k in range(8):
                        p.mm(py[:], y2T[:, k, qt * 128:(qt + 1) * 128], wo[:, k, hh * 512:(hh + 1) * 512],
                             start=(k == 0), stop=(k == 7), r=["wo"] + [("y2T", k, qt // 4)], w=[pyk])
                    p.copy("act" if hh == 0 else "dve", y_[:, hh * 512:(hh + 1) * 512], py[:], r=[pyk], w=[(yk, hh)])
                p.dma("sp", ymix[qt * 128:(qt + 1) * 128, :], y_[:], r=[(yk, 0), (yk, 1)], w=[("ymix", qt)])


def s5_inputs(inp, j, hf):
    f = np.float32
    order = [0, 1] if hf == 0 else [1, 0]
    are = inp["s5_a_re"][j][order]
    aim = inp["s5_a_im"][j][order]
    ldt = inp["s5_log_dt"][j][order]
    bre = inp["s5_b_re"][j][order]
    bim = inp["s5_b_im"][j][order]
    cre = inp["s5_c_re"][j][order]
    cim = inp["s5_c_im"][j][order]
    d = {}
    d["s5_are"] = np.ascontiguousarray(np.broadcast_to(are[:, None], (2, 128, 64, 64))).astype(f)
    d["s5_aim"] = np.ascontiguousarray(np.broadcast_to(aim[:, None], (2, 128, 64, 64))).astype(f)
    d["s5_ldt"] = np.ascontiguousarray(np.broadcast_to(ldt[:, None], (2, 128, 64))).astype(f)
    bt = bre.transpose(0, 3, 1, 2)
    d["s5_bre"] = np.ascontiguousarray(np.tile(bt, (1, 8, 1, 1))).astype(f)
    bt = bim.transpose(0, 3, 1, 2)
    d["s5_bim"] = np.ascontiguousarray(np.tile(bt, (1, 8, 1, 1))).astype(f)
    crt = cre.transpose(0, 3, 1, 2)
    cit = cim.transpose(0, 3, 1, 2)
    d["s5_c1"] = np.ascontiguousarray(np.concatenate([crt, cit], axis=1)).astype(f)
    d["s5_c2"] = np.ascontiguousarray(np.concatenate([cit, crt], axis=1)).astype(f)
    col = np.stack([are.transpose(0, 2, 1), aim.transpose(0, 2, 1),
                    np.broadcast_to(ldt[:, None, :], (2, 64, 64))], axis=2)
    d["s5_colp"] = np.ascontiguousarray(np.concatenate([col, col], axis=1)).astype(f)
    g = np.arange(64)
    jj = np.arange(8)
    mB = (g[None, :] % 8 == (np.arange(128) // 16)[:, None]).astype(f)
    d["s5_maskB"] = mB
    mJ = (g[:, None] % 8 == jj[None, :]).astype(f)
    d["s5_maskJ"] = np.ascontiguousarray(np.broadcast_to(mJ[None], (128, 64, 8))).astype(f)
    sgn = np.ones((128, 2), f)
    sgn[64:, 0] = -1.0
    sgn[:, 1] = -1.0
    d["s5_sgn"] = sgn
    d["s5_d"] = np.ascontiguousarray(inp["s5_d"][j].reshape(8, 128).T).astype(f)
    d["w_glu"] = inp["s5_w_glu"][j]
    d["w_so"] = inp["s5_w_out"][j]
    return d


LAYER_INPUTS = {
    "common": [("cT", [128, 8], F32), ("ada_w", [D_MODEL, 6 * D_MODEL], F32), ("ada_b", [1, 6 * D_MODEL], F32),
               ("pln", [4, 128, D_MODEL], F32), ("w_r", [D_MODEL, NEXP], F32), ("b_r", [1, NEXP], F32),
               ("w_gu", [NEXP, D_MODEL, 2 * D_MODEL], F32), ("b_guT", [128, NEXP, 16], F32),
               ("w_down", [NEXP, D_MODEL, D_MODEL], F32), ("b_down", [NEXP, D_MODEL], F32)],
    0: [("dftL", [4, 8, 128, 4, 2, 512], BF16), ("dftC", [128, 2, 2, 256], BF16), ("fnet_w", [D_MODEL, D_MODEL], F32),
        ("fnet_b", [1, D_MODEL], F32)],
    1: [("w_qkv", [D_MODEL, 2048], F32), ("g12", [128, 12, 128], F32), ("rope", [32, 128, 2, 128], F32),
        ("w_o", [D_MODEL, D_MODEL], F32)],
    2: [("s5_are", [2, 128, 64, 64], F32), ("s5_aim", [2, 128, 64, 64], F32), ("s5_ldt", [2, 128, 64], F32),
        ("s5_bre", [2, 128, 64, 64], F32), ("s5_bim", [2, 128, 64, 64], F32), ("s5_c1", [2, 128, 64, 16], F32),
        ("s5_c2", [2, 128, 64, 16], F32), ("s5_colp", [2, 128, 3, 64], F32), ("s5_maskB", [128, 64], F32),
        ("s5_maskJ", [128, 64, 8], F32), ("s5_sgn", [128, 2], F32), ("s5_d", [128, 8], F32),
        ("w_glu", [D_MODEL, D_MODEL], F32), ("w_so", [D_MODEL, D_MODEL], F32)],
    3: [("w_in", [D_MODEL, 5, D_MODEL], F32), ("lbT", [128, 4, 8], F32), ("cmask", [128, HALF], F32),
        ("tri", [64, 2, 64], F32), ("hnorm", [64, 8, 128], F32), ("w_ho", [D_MODEL, D_MODEL], F32)],
}


def phase_local_exchange(c, outs, xps, li):
    p = c.p
    with p.scope():
        a_ = Rot(p, "xa", [128, 1024], F32, 3)
        o_ = Rot(p, "xo", [128, 1024], F32, 3)
        ps = Rot(p, "xps", [128, 512], F32, 4, psum=True)
        for v in range(2):
            for t in range(16):
                src0 = HALF - 128 * (t + 1)
                a, ak = a_.next()
                p.dma("sp", a[:], outs[1 - v][src0:src0 + 128, :], w=[ak])
                o, ok = o_.next()
                for hh in range(2):
                    q, qk = ps.next()
                    p.mm(q[:], c.jrev[:], a[:, hh * 512:(hh + 1) * 512], r=[ak], w=[qk])
                    p.copy("act" if hh == 0 else "dve", o[:, hh * 512:(hh + 1) * 512], q[:], r=[qk], w=[(ok, hh)])
                p.dma("sp", xps[v][HALF + t * 128:HALF + (t + 1) * 128, :], o[:], r=[(ok, 0), (ok, 1)], w=[("xpn", v, t)])
            p.dma("act", xps[v][0:HALF, :], outs[v][:, :], w=[("xpn0", v)])


HF_DEP = {0: ["dftL"], 1: ["rope"], 2: [], 3: []}


def build_fused(depth=DEPTH):
    nc = bass.Bass("TRN2", target_bir_lowering=False)
    c = Ctx()
    c.nc = nc
    Dall = {}
    c.Dall = Dall

    def din(name, shape, dt):
        Dall[name] = nc.dram_tensor(name, list(shape), dt, kind="ExternalInput").ap()
    din("xp_v0", [SEQ, D_MODEL], F32)
    din("xp_v1", [SEQ, D_MODEL], F32)
    for li in range(depth):
        m = li % 4
        for name, shape, dt in LAYER_INPUTS["common"] + LAYER_INPUTS[m]:
            if name in HF_DEP[m]:
                for v in range(2):
                    din("%s_L%d_v%d" % (name, li, v), shape, dt)
            else:
                din("%s_L%d" % (name, li), shape, dt)
    out = nc.dram_tensor("out", [2, HALF, D_MODEL], F32, kind="ExternalOutput").ap()
    c.p = Prog(nc)
    setup_consts(c)
    p = c.p
    c.jrev = p.sbuf("jrev", [128, 128], F32)
    p.memset("dve", c.jrev[:], 0.0, w=["jrev"])
    p.op("pool", (lambda e: e.affine_select(out=c.jrev[:], in_=c.jrev[:], pattern=[[1, 128]], compare_op=ALU.not_equal,
                                            fill=1.0, base=-127, channel_multiplier=1)), r=["jrev"], w=["jrev"])
    p.flush()
    xps = [Dall["xp_v0"], Dall["xp_v1"]]
    for li in range(depth):
        m = li % 4
        last = li == depth - 1
        outs = [out[v] if last else nc.dram_tensor("xown%d_%d" % (li, v), [HALF, D_MODEL], F32).ap() for v in range(2)]
        c.modd = nc.dram_tensor("modd%d" % li, [128, 6 * D_MODEL], F32).ap()
        p.new_epoch()
        for v in range(2):
            D = {}
            for name, shape, dt in LAYER_INPUTS["common"] + LAYER_INPUTS[m]:
                D[name] = Dall["%s_L%d_v%d" % (name, li, v)] if name in HF_DEP[m] else Dall["%s_L%d" % (name, li)]
            D["xp"] = xps[v]
            D["out"] = outs[v]
            c.D = D
            c.vhf = v
            sfx = "%d_%d" % (li, v)
            c.lsuffix = sfx
            c.x1buf = nc.dram_tensor("x1buf" + sfx, [HALF, D_MODEL], F32).ap()
            c.h2T = nc.dram_tensor("h2T" + sfx, [128, 8, HALF], BF16).ap()
            c.gbuf = nc.dram_tensor("gbuf" + sfx, [16, 128, NEXP], F32).ap()
            ymix = nc.dram_tensor("ymix" + sfx, [HALF, D_MODEL], F32).ap()
            if v == 0:
                phase_mod(c)
            if m == 0:
                phase_fnet(c, ymix)
            elif m == 1:
                phase_attn(c, ymix)
            elif m == 2:
                phase_s5(c, ymix)
            else:
                phase_hgrn(c, ymix, li)
            phase_post(c, ymix)
            phase_moe(c)
        if not last:
            nxt = [nc.dram_tensor("xpn%d_%d" % (li, v), [SEQ, D_MODEL], F32).ap() for v in range(2)]
            phase_local_exchange(c, outs, nxt, li)
            xps = nxt
    c.p.finish()
    c.n_ops = c.p.total_ops
    return nc, c


_FUSED = []


def layer_inputs(inp, li, b, hf, cache):
    m, j = li % 4, li // 4
    key = ("common", li, b)
    if key not in cache:
        cache[key] = common_inputs(inp, li, b)
    d = dict(cache[key])
    if m == 0:
        if ("dft", hf) not in cache:
            cache[("dft", hf)] = fnet_tables(hf)
            cache["dftC"] = fnet_ctab()
        d["dftL"] = cache[("dft", hf)]
        d["dftC"] = cache["dftC"]
        d["fnet_w"] = inp["fnet_w_out"][j]
        d["fnet_b"] = inp["fnet_b_out"][j][None, :]
    elif m == 1:
        d["w_qkv"] = inp["attn_w_qkv"][j]
        g = np.concatenate([np.tile(inp["attn_q_norm"][j][None, :], (8, 1)), np.tile(inp["attn_k_norm"][j][None, :], (4, 1))])
        d["g12"] = np.ascontiguousarray(np.broadcast_to(g[None], (128, 12, 128))).astype(np.float32)
        d["rope"] = rope_tables(hf)
        d["w_o"] = inp["attn_w_out"][j]
    elif m == 2:
        d.update(s5_inputs(inp, j, hf))
    else:
        w = inp["hg_w_in"][j].reshape(D_MODEL, 5, D_MODEL)
        if hf == 1:
            w = w[:, [0, 2, 1, 3, 4], :]
        d["w_in"] = np.ascontiguousarray(w)
        d["lbT"] = np.ascontiguousarray(inp["hg_lb"].reshape(4, 8, 128).transpose(2, 0, 1))
        cmk = np.ones((128, HALF), np.float32)
        cmk[:, ::64] = 0.0
        d["cmask"] = cmk
        sidx = np.arange(64)
        d["tri"] = np.stack([(sidx[:, None] <= sidx[None, :]), (sidx[:, None] >= sidx[None, :])], axis=1).astype(np.float32)
        d["hnorm"] = np.ascontiguousarray(np.broadcast_to(inp["hg_norm"][j].reshape(1, 8, 128), (64, 8, 128))).astype(np.float32)
        d["w_ho"] = inp["hg_w_out"][j]
    return d


def kernel_fused(inp, depth=DEPTH):
    if not _FUSED:
        _FUSED.append(build_fused(depth))
    nc, c = _FUSED[0]
    x = np.asarray(inp["x"], dtype=np.float32)
    cache = {}
    in_maps = []
    NB = x.shape[0]
    for b in range(NB):
        d = {"xp_v0": present(x[b], 0), "xp_v1": present(x[b], 1)}
        for li in range(depth):
            m = li % 4
            per_v = [layer_inputs(inp, li, b, v, cache) for v in range(2)] if HF_DEP[m] else None
            base = layer_inputs(inp, li, b, 0, cache)
            for k, v_ in base.items():
                if k in HF_DEP[m]:
                    for v in range(2):
                        d["%s_L%d_v%d" % (k, li, v)] = per_v[v][k]
                else:
                    d["%s_L%d" % (k, li)] = v_
        in_maps.append(d)
    res = run_bass_kernel_spmd(nc, in_maps, core_ids=list(range(NB)))
    out = np.empty_like(x)
    for b in range(NB):
        o = res.results[b]["out"]
        out[b, unpresent_idx(0)] = o[0]
        out[b, unpresent_idx(1)] = o[1]
    return out


def kernel(**inputs):
    inp = {k: np.asarray(v) for k, v in inputs.items()}
    return kernel_fused(inp)
```

```python
from contextlib import ExitStack, contextmanager
import math
import numpy as np
import ml_dtypes
import concourse.bass as bass
import concourse.mybir as mybir
from concourse.bass_utils import run_bass_kernel_spmd

F32 = mybir.dt.float32
BF16 = mybir.dt.bfloat16
I32 = mybir.dt.int32
ALU = mybir.AluOpType
ACT = mybir.ActivationFunctionType
AX = mybir.AxisListType

NDMASEM = 8
D_MODEL = 1024
SEQ = 4096
HALF = 2048
NEXP = 32
DEPTH = 4
ALPHA = (2 * DEPTH) ** 0.25
LN_EPS = 1e-5
RMS_EPS = 1e-6


class Prog:
    ENGS = ("pe", "dve", "act", "pool", "sp")

    def __init__(self, nc):
        self.nc = nc
        self.ops = []
        self.last_w = {}
        self.readers = {}
        self.gstack = ExitStack()
        self.stacks = [self.gstack]
        self.sems = {e: self.gstack.enter_context(nc.semaphore("s_" + e)) for e in ("pe", "dve", "act", "pool")}
        self.dsems = {e: [self.gstack.enter_context(nc.semaphore("d_%s%d" % (e, j))) for j in range(NDMASEM)]
                      for e in ("sp", "act", "pool")}
        self.cnt = {e: 0 for e in self.ENGS}
        self.dcnt = {e: 0 for e in self.ENGS}
        self.waited = {e: {} for e in self.ENGS}
        self.total_ops = 0

    def sbuf(self, name, shape, dtype):
        self.uid = getattr(self, "uid", 0) + 1
        return self.stacks[-1].enter_context(self.nc.sbuf_tensor("sb%d_%s" % (self.uid, name), list(shape), dtype))

    def psum(self, name, shape, dtype=F32):
        self.uid = getattr(self, "uid", 0) + 1
        return self.stacks[-1].enter_context(self.nc.psum_tensor("ps%d_%s" % (self.uid, name), list(shape), dtype))

    @contextmanager
    def scope(self):
        st = ExitStack()
        self.stacks.append(st)
        yield
        self.flush()
        self.stacks.pop()
        st.close()

    def op(self, eng, fn, r=(), w=(), dma=False):
        i = len(self.ops)
        deps = set()
        for k in r:
            if k in self.last_w:
                deps.add(self.last_w[k])
        for k in w:
            if k in self.last_w:
                deps.add(self.last_w[k])
            for q in self.readers.get(k, ()):
                deps.add(q)
        for k in w:
            self.last_w[k] = i
            self.readers[k] = []
        for k in r:
            if k not in w:
                self.readers.setdefault(k, []).append(i)
        deps.discard(i)
        self.ops.append(dict(eng=eng, fn=fn, deps=deps, dma=dma))
        return i

    def dma(self, q, out, in_, r=(), w=(), **kw):
        def fn(e):
            return e.dma_start(out=out, in_=in_, **kw)
        return self.op(q, fn, r=r, w=w, dma=True)

    def mm(self, out, lhsT, rhs, start=True, stop=True, r=(), w=()):
        def fn(e):
            return e.matmul(out, lhsT, rhs, start=start, stop=stop)
        rr = list(r)
        if not start:
            rr = rr + list(w)
        return self.op("pe", fn, r=rr, w=w)

    def act(self, out, in_, func, r=(), w=(), **kw):
        def fn(e):
            return e.activation(out=out, in_=in_, func=func, **kw)
        return self.op("act", fn, r=r, w=w)

    def tt(self, eng, out, in0, in1, op, r=(), w=()):
        def fn(e):
            return e.tensor_tensor(out=out, in0=in0, in1=in1, op=op)
        return self.op(eng, fn, r=r, w=w)

    def ts(self, eng, out, in0, s1, op0, s2=None, op1=None, r=(), w=(), **kw):
        def fn(e):
            if op1 is None:
                return e.tensor_scalar(out=out, in0=in0, scalar1=s1, scalar2=None, op0=op0, **kw)
            return e.tensor_scalar(out=out, in0=in0, scalar1=s1, scalar2=s2, op0=op0, op1=op1, **kw)
        return self.op(eng, fn, r=r, w=w)

    def stt(self, eng, out, in0, scalar, in1, op0, op1, r=(), w=()):
        def fn(e):
            return e.scalar_tensor_tensor(out=out, in0=in0, scalar=scalar, in1=in1, op0=op0, op1=op1)
        return self.op(eng, fn, r=r, w=w)

    def copy(self, eng, out, in_, r=(), w=()):
        if eng == "act":
            def fn(e):
                return e.copy(out=out, in_=in_)
        else:
            def fn(e):
                return e.tensor_copy(out=out, in_=in_)
        return self.op(eng, fn, r=r, w=w)

    def memset(self, eng, ap, val, w=()):
        def fn(e):
            return e.memset(ap, val)
        return self.op(eng, fn, w=w)

    def flush(self):
        nc = self.nc
        ops = self.ops
        if not ops:
            return
        engs = {}
        for i, o in enumerate(ops):
            engs.setdefault(o["eng"], []).append(i)

        def skip(p, o):
            return (not p["dma"]) and (not o["dma"]) and p["eng"] == o["eng"] == "pe"
        needed = set()
        for i, o in enumerate(ops):
            best = {}
            keep = set()
            for d in o["deps"]:
                pd = ops[d]
                if skip(pd, o):
                    continue
                if pd["dma"]:
                    keep.add(d)
                else:
                    if pd["eng"] not in best or best[pd["eng"]] < d:
                        best[pd["eng"]] = d
            keep.update(best.values())
            o["deps"] = keep
            needed.update(keep)
        for i, o in enumerate(ops):
            e = o["eng"]
            o["prewait"] = None
            if o["dma"]:
                k = self.dcnt[e]
                self.dcnt[e] += 1
                s = self.dsems[e][k % NDMASEM]
                o["sig"] = (s, 16 * (k // NDMASEM + 1), 16)
                if k >= NDMASEM:
                    o["prewait"] = (s, 16 * (k // NDMASEM))
            elif i in needed:
                self.cnt[e] += 1
                o["sig"] = (self.sems[e], self.cnt[e], 1)
            else:
                o["sig"] = None
        for e, lst in engs.items():
            waited = self.waited[e]
            for i in lst:
                o = ops[i]
                ws = {}
                if o["prewait"] is not None:
                    s_, v = o["prewait"]
                    ws[id(s_)] = (s_, v)
                for d in o["deps"]:
                    p = ops[d]
                    if skip(p, o):
                        continue
                    s_, v, _ = p["sig"]
                    if id(s_) not in ws or ws[id(s_)][1] < v:
                        ws[id(s_)] = (s_, v)
                o["waits"] = []
                for kk, (s_, v) in ws.items():
                    if waited.get(kk, 0) < v:
                        waited[kk] = v
                        o["waits"].append((s_, v))
        finals = {}
        for e in engs:
            if e in self.dsems:
                n = self.dcnt[e]
                fl = []
                for j in range(NDMASEM):
                    c = (n - j + NDMASEM - 1) // NDMASEM if n > j else 0
                    if c > 0 and self.waited[e].get(id(self.dsems[e][j]), 0) < 16 * c:
                        self.waited[e][id(self.dsems[e][j])] = 16 * c
                        fl.append((self.dsems[e][j], 16 * c))
                finals[e] = fl
        self.total_ops += len(ops)
        with nc.Block() as block:
            def mk(e):
                lst = engs[e]

                def body(eng):
                    for i in lst:
                        o = ops[i]
                        for s, v in o["waits"]:
                            eng.wait_ge(s, v)
                        inst = o["fn"](eng)
                        if o["sig"] is not None:
                            s, _, inc = o["sig"]
                            inst.then_inc(s, inc)
                    for s, v in finals.get(e, ()):
                        eng.wait_ge(s, v)
                return body
            reg = {"pe": block.tensor, "dve": block.vector, "act": block.scalar,
                   "pool": block.gpsimd, "sp": block.sync}
            for e in engs:
                reg[e](mk(e))
        nc.all_engine_barrier()
        self.ops = []
        self.last_w = {}
        self.readers = {}

    def new_epoch(self):
        self.flush()
        self.epoch = getattr(self, "epoch", 0) + 1
        self.sems = {e: self.gstack.enter_context(self.nc.semaphore("s%d_%s" % (self.epoch, e)))
                     for e in ("pe", "dve", "act", "pool")}
        for e in ("pe", "dve", "act", "pool"):
            self.cnt[e] = 0

    def finish(self):
        self.flush()
        self.gstack.close()


class Rot:
    def __init__(self, p, name, shape, dtype, n, psum=False):
        self.t = [(p.psum if psum else p.sbuf)("%s%d" % (name, i), shape, dtype) for i in range(n)]
        self.name = name
        self.i = -1

    def next(self):
        self.i += 1
        j = self.i % len(self.t)
        return self.t[j], (self.name, j)


class Ctx:
    pass


def setup_consts(c):
    p = c.p
    c.ident = p.sbuf("ident", [128, 128], F32)
    c.identb = p.sbuf("identb", [128, 128], BF16)
    c.ones = p.sbuf("ones", [128, 128], F32)
    p.memset("dve", c.ident[:], 0.0, w=["ident"])
    p.memset("dve", c.ones[:], 1.0, w=["ones"])

    def aff(e):
        return e.affine_select(out=c.ident[:], in_=c.ident[:], pattern=[[-1, 128]], compare_op=ALU.not_equal,
                               fill=1.0, base=0, channel_multiplier=1)
    p.op("pool", aff, r=["ident"], w=["ident"])
    p.copy("dve", c.identb[:], c.ident[:], r=["ident"], w=["identb"])
    p.flush()


def phase_mod(c):
    p, D = c.p, c.D
    with p.scope():
        cT = p.sbuf("cT", [128, 8], F32)
        cs = p.sbuf("cs", [128, 8], F32)
        crep = p.sbuf("crep", [128, 8, 128], F32)
        abias = p.sbuf("abias", [1, 6144], F32)
        aw = Rot(p, "aw", [128, 8, 512], F32, 2)
        mt = Rot(p, "mt", [128, 512], F32, 2)
        ps = Rot(p, "psm", [128, 512], F32, 2, psum=True)
        p.dma("sp", cT[:], D["cT"][:, :], w=["cT"])
        p.dma("sp", abias[:], D["ada_b"][:, :], w=["abias"])
        p.act(cs[:], cT[:], ACT.Silu, r=["cT"], w=["cs"])
        for k in range(8):
            p.ts("dve", crep[:, k, :], c.ones[:, :], cs[:, k:k + 1], ALU.mult, r=["cs"], w=[("crep", k)])
        for n in range(12):
            awt, awk = aw.next()
            pst, psk = ps.next()
            mtt, mtk = mt.next()
            p.dma("sp" if n % 2 == 0 else "act", awt[:],
                  D["ada_w"][:, n * 512:(n + 1) * 512].rearrange("(c p) n -> p c n", p=128), w=[awk])
            for k in range(8):
                p.mm(pst[:], crep[:, k, :], awt[:, k, :], start=(k == 0), stop=False, r=[("crep", k), awk], w=[psk])
            p.mm(pst[:], c.ones[0:1, :], abias[0:1, n * 512:(n + 1) * 512], start=False, stop=True, r=["abias"], w=[psk])
            if n in (2, 3, 8, 9):
                p.ts("dve", mtt[:], pst[:], 1.0, ALU.add, r=[psk], w=[mtk])
            else:
                p.copy("dve", mtt[:], pst[:], r=[psk], w=[mtk])
            p.dma("sp", c.modd[:, n * 512:(n + 1) * 512], mtt[:], r=[mtk], w=[("modd", n)])


class LN:
    def __init__(self, p, name, nbuf=2):
        self.p = p
        self.st = Rot(p, name + "_st", [128, 2, 6], F32, nbuf)
        self.mv = Rot(p, name + "_mv", [128, 2], F32, nbuf)
        self.rs = Rot(p, name + "_rs", [128, 1], F32, nbuf)
        self.xn = Rot(p, name + "_xn", [128, 1024], F32, nbuf)

    def __call__(self, xin, xk, out, outk, scale, shift, ck, eng2="pool", extra_r=()):
        p = self.p
        st, stk = self.st.next()
        mv, mvk = self.mv.next()
        rs, rsk = self.rs.next()
        xn, xnk = self.xn.next()
        for hh in range(2):
            def f(e, hh=hh):
                return e.bn_stats(out=st[:, hh, :], in_=xin[:, hh * 512:(hh + 1) * 512])
            p.op("dve", f, r=[xk], w=[(stk, hh)])

        def g(e):
            return e.bn_aggr(out=mv[:], in_=st[:])
        p.op("dve", g, r=[(stk, 0), (stk, 1)], w=[mvk])
        p.ts("dve", rs[:], mv[:, 1:2], LN_EPS, ALU.add, r=[mvk], w=[rsk])
        p.op("act", (lambda e, rs=rs: e.sqrt(out=rs[:], in_=rs[:])), r=[rsk], w=[rsk])
        p.op("dve", (lambda e, rs=rs: e.reciprocal(out=rs[:], in_=rs[:])), r=[rsk], w=[rsk])
        p.ts("dve", xn[:], xin, mv[:, 0:1], ALU.subtract, rs[:, 0:1], ALU.mult, r=[xk, mvk, rsk], w=[xnk])
        p.tt(eng2, xn[:], xn[:], scale, ALU.mult, r=[xnk] + list(ck), w=[xnk])
        p.tt(eng2, out, xn[:], shift, ALU.add, r=[xnk] + list(ck) + list(extra_r), w=[outk])


def phase_post(c, ymix):
    p, D = c.p, c.D
    with p.scope():
        mods = p.sbuf("modsB", [128, 3, 1024], F32)
        pl = p.sbuf("plB", [128, 2, 1024], F32)
        wr = p.sbuf("wr", [128, 8, 32], F32)
        br = p.sbuf("br", [1, 32], F32)
        p.dma("sp", mods[:, 0, :], c.modd[:, 2048:3072], w=["mods"])
        p.dma("sp", mods[:, 1, :], c.modd[:, 3072:4096], w=["mods"])
        p.dma("sp", mods[:, 2, :], c.modd[:, 4096:5120], w=["mods"])
        p.dma("act", pl[:, 0, :], D["pln"][0], w=["pl"])
        p.dma("act", pl[:, 1, :], D["pln"][1], w=["pl"])
        p.dma("act", wr[:], D["w_r"].rearrange("(c p) n -> p c n", p=128), w=["wr"])
        p.dma("act", br[:], D["b_r"][:, :], w=["br"])
        xt = Rot(p, "xtB", [128, 1024], F32, 2)
        yt = Rot(p, "ytB", [128, 1024], F32, 2)
        x1 = Rot(p, "x1B", [128, 1024], F32, 2)
        h2 = Rot(p, "h2B", [128, 1024], F32, 2)
        hTf = Rot(p, "hTf", [128, 8, 128], F32, 2)
        hTb = Rot(p, "hTb", [128, 8, 128], BF16, 2)
        pst = Rot(p, "pstB", [128, 4, 128], F32, 4, psum=True)
        psl = Rot(p, "pslB", [128, 512], F32, 2, psum=True)
        lg = Rot(p, "lg", [128, 32], F32, 2)
        m8 = Rot(p, "m8", [128, 8], F32, 2)
        nm = Rot(p, "nm", [128, 1], F32, 2)
        msk = Rot(p, "msk", [128, 32], F32, 2)
        ex = Rot(p, "ex", [128, 32], F32, 2)
        sm = Rot(p, "sm", [128, 1], F32, 2)
        gt = Rot(p, "gt", [128, 32], F32, 2)
        ln1 = LN(p, "lnB1")
        ln2 = LN(p, "lnB2")
        for t in range(16):
            x, xk = xt.next()
            y, yk = yt.next()
            p.dma("sp", x[:], D["xp"][t * 128:(t + 1) * 128, :], w=[xk])
            p.dma("act", y[:], ymix[t * 128:(t + 1) * 128, :], w=[yk])
            p.tt("pool", y[:], y[:], mods[:, 0, :], ALU.mult, r=[yk, "mods"], w=[yk])
            p.stt("dve", y[:], x[:], ALPHA, y[:], ALU.mult, ALU.add, r=[xk, yk], w=[yk])
            a, ak = x1.next()
            ln1(y[:], yk, a[:], ak, pl[:, 0, :], pl[:, 1, :], ["pl"])
            p.dma("sp", c.x1buf[t * 128:(t + 1) * 128, :], a[:], r=[ak], w=[("x1buf", t)])
            import os
            KP = int(os.environ.get("KPOST", "9"))
            if KP < 2:
                continue
            h, hk = h2.next()
            ln2(a[:], ak, h[:], hk, mods[:, 2, :], mods[:, 1, :], ["mods"])
            hf_, hfk = hTf.next()
            hb_, hbk = hTb.next()
            if os.environ.get("KV", "") == "noT":
                continue
            for q in range(2):
                ps, psk = pst.next()
                for k in range(4):
                    p.mm(ps[:, k, :], h[:, (q * 4 + k) * 128:(q * 4 + k + 1) * 128], c.ident[:], r=[hk], w=[(psk, k)])
                pk = [(psk, k) for k in range(4)]
                p.copy("act", hf_[:, q * 4:(q + 1) * 4, :], ps[:], r=pk, w=[(hfk, q)])
                p.copy("pool", hb_[:, q * 4:(q + 1) * 4, :], hf_[:, q * 4:(q + 1) * 4, :], r=[(hfk, q)], w=[(hbk, q)])
            hfk2 = [(hfk, 0), (hfk, 1)]
            hbk2 = [(hbk, 0), (hbk, 1)]
            if os.environ.get("KV", "") != "nodma":
                p.dma("sp", c.h2T[:, :, t * 128:(t + 1) * 128], hb_[:], r=hbk2, w=[("h2T", t)])
            if KP < 3:
                continue
            pl_, plk = psl.next()
            for k in range(8):
                p.mm(pl_[:, 0:32], hf_[:, k, :], wr[:, k, :], start=(k == 0), stop=False, r=hfk2 + ["wr"], w=[plk])
            p.mm(pl_[:, 0:32], c.ones[0:1, :], br[0:1, :], start=False, stop=True, r=["br"], w=[plk])
            l, lk = lg.next()
            p.copy("dve", l[:], pl_[:, 0:32], r=[plk], w=[lk])
            if KP < 4:
                continue
            m, mk_ = m8.next()

            def fmax(e, m=m, l=l):
                return e.max(out=m[:], in_=l[:])
            p.op("dve", fmax, r=[lk], w=[mk_])
            n_, nk = nm.next()
            p.ts("dve", n_[:], m[:, 0:1], -1.0, ALU.mult, r=[mk_], w=[nk])
            ms, msk_ = msk.next()
            p.ts("dve", ms[:], l[:], m[:, 3:4], ALU.is_ge, r=[lk, mk_], w=[msk_])
            e_, ek = ex.next()
            p.act(e_[:], l[:], ACT.Exp, bias=n_[:, 0:1], scale=1.0, r=[lk, nk], w=[ek])
            p.tt("dve", e_[:], e_[:], ms[:], ALU.mult, r=[ek, msk_], w=[ek])
            s_, sk = sm.next()

            def fsum(e, s_=s_, e_=e_):
                return e.reduce_sum(out=s_[:], in_=e_[:], axis=AX.X)
            p.op("dve", fsum, r=[ek], w=[sk])
            g_, gk = gt.next()
            p.op("dve", (lambda e, s_=s_: e.reciprocal(out=s_[:], in_=s_[:])), r=[sk], w=[sk])
            p.ts("dve", g_[:], e_[:], s_[:, 0:1], ALU.mult, r=[ek, sk], w=[gk])
            p.dma("sp", c.gbuf[t], g_[:], r=[gk], w=[("gbuf", t)])


def phase_moe(c):
    p, D = c.p, c.D
    for tp in range(2):
        with p.scope():
            hT = p.sbuf("hTm", [128, 8, 1024], BF16)
            G = p.sbuf("Gm", [128, 8, 32], F32)
            acc = p.sbuf("accm", [128, 8, 1024], F32)
            bgu = p.sbuf("bgu", [128, NEXP, 16], F32)
            p.dma("sp", hT[:], c.h2T[:, :, tp * 1024:(tp + 1) * 1024], w=["hT"])
            p.dma("sp", G[:], c.gbuf[tp * 8:(tp + 1) * 8].rearrange("t p e -> p t e"), w=["G"])
            p.dma("sp", bgu[:], D["b_guT"][:, :, :], w=["bgu"])
            bgu1 = p.sbuf("bgu1", [128, NEXP, 16], F32)
            p.ts("dve", bgu1[:], bgu[:], 1.0, ALU.add, r=["bgu"], w=["bgu1"])
            F7 = 7.0 / (1.0 + math.exp(-1.702 * 7.0))
            with p.scope():
                wgu = Rot(p, "wgu", [128, 8, 2048], BF16, 2)
                wd = Rot(p, "wd", [128, 8, 1024], BF16, 2)
                bd = Rot(p, "bd", [1, 1024], F32, 2)
                psg = Rot(p, "psg", [128, 512], F32, 3, psum=True)
                psu = Rot(p, "psu", [128, 512], F32, 3, psum=True)
                pso = Rot(p, "pso", [128, 512], F32, 2, psum=True)
                gs = Rot(p, "gs", [128, 512], F32, 4)
                sg = Rot(p, "sg", [128, 512], F32, 4)
                us = Rot(p, "us", [128, 512], F32, 4)
                aT = Rot(p, "aT", [128, 8, 512], BF16, 2)
                for e in range(NEXP):
                    wg_, wgk = wgu.next()
                    wd_, wdk = wd.next()
                    bd_, bdk = bd.next()
                    p.dma("pool", wg_[:], D["w_gu"][e].rearrange("(c p) n -> p c n", p=128), w=[wgk])
                    p.dma("pool", wd_[:], D["w_down"][e].rearrange("(c p) n -> p c n", p=128), w=[wdk])
                    p.dma("sp", bd_[:], D["b_down"][e:e + 1, :], w=[bdk])
                    for blk in range(2):
                        a_, aTk = aT.next()
                        for j in range(8):
                            pg, pgk = psg.next()
                            pu, puk = psu.next()
                            for k in range(8):
                                p.mm(pg[:], wg_[:, k, j * 128:(j + 1) * 128], hT[:, k, blk * 512:(blk + 1) * 512],
                                     start=(k == 0), stop=(k == 7), r=[wgk, "hT"], w=[pgk])
                            for k in range(8):
                                p.mm(pu[:], wg_[:, k, 1024 + j * 128:1024 + (j + 1) * 128], hT[:, k, blk * 512:(blk + 1) * 512],
                                     start=(k == 0), stop=(k == 7), r=[wgk, "hT"], w=[puk])
                            g_, gk = gs.next()
                            u_, uk = us.next()
                            p.act(g_[:], pg[:], ACT.Gelu_apprx_sigmoid, bias=bgu[:, e, j:j + 1], scale=1.0, r=[pgk, "bgu"], w=[gk])
                            p.act(u_[:], pu[:], ACT.Identity, bias=bgu1[:, e, 8 + j:9 + j], scale=1.0, r=[puk, "bgu1"], w=[uk])
                            p.ts("dve", u_[:], u_[:], 8.0, ALU.min, -6.0, ALU.max, r=[uk], w=[uk])
                            p.stt("dve", a_[:, j, :], g_[:], F7, u_[:], ALU.min, ALU.mult, r=[gk, uk], w=[(aTk, j)])
                        for tl in range(4):
                            tt_ = blk * 4 + tl
                            for hh in range(2):
                                po, pok = pso.next()
                                for j in range(8):
                                    p.mm(po[:], a_[:, j, tl * 128:(tl + 1) * 128], wd_[:, j, hh * 512:(hh + 1) * 512],
                                         start=(j == 0), stop=False, r=[(aTk, j), wdk], w=[pok])
                                p.mm(po[:], c.ones[0:1, :], bd_[0:1, hh * 512:(hh + 1) * 512], start=False, stop=True,
                                     r=[bdk], w=[pok])
                                dst = acc[:, tt_, hh * 512:(hh + 1) * 512]
                                ak = ("acc", tt_, hh)
                                if e == 0:
                                    p.ts("dve", dst, po[:], G[:, tt_, e:e + 1], ALU.mult, r=[pok, "G"], w=[ak])
                                else:
                                    p.stt("dve", dst, po[:], G[:, tt_, e:e + 1], dst, ALU.mult, ALU.add,
                                          r=[pok, "G", ak], w=[ak])
            g2 = p.sbuf("g2m", [128, 1024], F32)
            pl = p.sbuf("plm", [128, 2, 1024], F32)
            p.dma("sp", g2[:], c.modd[:, 5120:6144], w=["g2"])
            p.dma("sp", pl[:, 0, :], D["pln"][2], w=["plm"])
            p.dma("sp", pl[:, 1, :], D["pln"][3], w=["plm"])
            x1 = Rot(p, "x1m", [128, 1024], F32, 2)
            ot = Rot(p, "otm", [128, 1024], F32, 2)
            ln = LN(p, "lnM")
            for tl in range(8):
                t = tp * 8 + tl
                a, ak = x1.next()
                p.dma("sp", a[:], c.x1buf[t * 128:(t + 1) * 128, :], w=[ak])
                z = acc[:, tl, :]
                zk = [("acc", tl, 0), ("acc", tl, 1)]
                p.tt("pool", z, z, g2[:], ALU.mult, r=zk + ["g2"], w=zk)
                p.stt("dve", a[:], a[:], ALPHA, z, ALU.mult, ALU.add, r=[ak] + zk, w=[ak])
                o, ok = ot.next()
                ln(a[:], ak, o[:], ok, pl[:, 0, :], pl[:, 1, :], ["plm"])
                p.dma("sp", D["out"][t * 128:(t + 1) * 128, :], o[:], r=[ok], w=[("out", t)])


def phase_fnet(c, ymix):
    p, D = c.p, c.D
    with p.scope():
        hall = p.sbuf("hall", [128, 32, 1024], BF16)
        with p.scope():
            mods = p.sbuf("modsA", [128, 2, 1024], F32)
            p.dma("sp", mods[:, 0, :], c.modd[:, 0:1024], w=["mods"])
            p.dma("sp", mods[:, 1, :], c.modd[:, 1024:2048], w=["mods"])
            xt = Rot(p, "xtA", [128, 1024], F32, 3)
            ln = LN(p, "lnA")
            for t in range(32):
                x, xk = xt.next()
                p.dma("sp" if t % 2 == 0 else "act", x[:], D["xp"][t * 128:(t + 1) * 128, :], w=[xk])
                ln(x[:], xk, hall[:, t, :], ("hall", t), mods[:, 1, :], mods[:, 0, :], ["mods"])
        abT = p.sbuf("abT", [128, 2, 8, HALF], BF16)
        with p.scope():
            tab = Rot(p, "tab", [128, 4, 2, 512], BF16, 3)
            psa = Rot(p, "psa", [128, 512], F32, 8, psum=True)
            qi = 0
            for oc in range(4):
                for cp in range(4):
                    accs = [psa.next() for _ in range(4)]
                    for l4 in range(8):
                        tb, tbk = tab.next()
                        p.dma("sp" if qi % 2 == 0 else "act", tb[:], D["dftL"][oc, l4], w=[tbk])
                        qi += 1
                        for li in range(4):
                            lt = l4 * 4 + li
                            for ci in range(2):
                                ch = cp * 2 + ci
                                for cs in range(2):
                                    pt, pk = accs[ci * 2 + cs]
                                    p.mm(pt[:], hall[:, lt, ch * 128:(ch + 1) * 128], tb[:, li, cs, :],
                                         start=(lt == 0), stop=(lt == 31), r=[("hall", lt), tbk], w=[pk])
                    for ci in range(2):
                        ch = cp * 2 + ci
                        for cs in range(2):
                            pt, pk = accs[ci * 2 + cs]
                            dst = abT[:, cs, ch, oc * 512:(oc + 1) * 512]
                            if cs == 0:
                                p.op("act", (lambda e, dst=dst, pt=pt: e.mul(out=dst, in_=pt[:], mul=1.0 / 1024.0)), r=[pk], w=[("abT", cs, ch, oc)])
                            else:
                                p.ts("dve", dst, pt[:], 1.0 / 1024.0, ALU.mult, r=[pk], w=[("abT", cs, ch, oc)])
        with p.scope():
            cc = p.sbuf("ccs", [128, 2, 2, 256], BF16)
            p.dma("sp", cc[:], D["dftC"][:, :, :, :], w=["cc"])
            wo = p.sbuf("woA", [128, 8, 1024], BF16)
            p.dma("pool", wo[:], D["fnet_w"].rearrange("(c p) n -> p c n", p=128), w=["wo"])
            bo = p.sbuf("boA", [1, 1024], F32)
            p.dma("sp", bo[:], D["fnet_b"][:, :], w=["bo"])
            yT = Rot(p, "yTA", [128, 8, 512], BF16, 2)
            psy = Rot(p, "psy", [128, 512], F32, 2, psum=True)
            pso = Rot(p, "psoA", [128, 512], F32, 2, psum=True)
            yo = Rot(p, "yoA", [128, 1024], F32, 2)
            for oc in range(4):
                y_, yk = yT.next()
                for kc in range(8):
                    g, kk = kc // 2, kc % 2
                    ps, psk = psy.next()
                    n = 0
                    for cs in range(2):
                        for cch in range(2):
                            p.mm(ps[:], cc[:, cch, cs, kk * 128:(kk + 1) * 128],
                                 abT[:, cs, g * 2 + cch, oc * 512:(oc + 1) * 512],
                                 start=(n == 0), stop=(n == 3), r=["cc", ("abT", cs, g * 2 + cch, oc)], w=[psk])
                            n += 1
                    if kc % 2 == 0:
                        p.copy("act", y_[:, kc, :], ps[:], r=[psk], w=[(yk, kc)])
                    else:
                        p.copy("dve", y_[:, kc, :], ps[:], r=[psk], w=[(yk, kc)])
                for tl in range(4):
                    t = oc * 4 + tl
                    o, ok = yo.next()
                    for hh in range(2):
                        po, pok = pso.next()
                        for kc in range(8):
                            p.mm(po[:], y_[:, kc, tl * 128:(tl + 1) * 128], wo[:, kc, hh * 512:(hh + 1) * 512],
                                 start=(kc == 0), stop=False, r=[(yk, kc), "wo"], w=[pok])
                        p.mm(po[:], c.ones[0:1, :], bo[0:1, hh * 512:(hh + 1) * 512], start=False, stop=True,
                             r=["bo"], w=[pok])
                        if hh == 0:
                            p.copy("act", o[:, 0:512], po[:], r=[pok], w=[(ok, 0)])
                        else:
                            p.copy("dve", o[:, 512:1024], po[:], r=[pok], w=[(ok, 1)])
                    p.dma("sp", ymix[t * 128:(t + 1) * 128, :], o[:], r=[(ok, 0), (ok, 1)], w=[("ymix", t)])


def build_layer(m):
    nc = bass.Bass("TRN2", target_bir_lowering=False)
    c = Ctx()
    c.nc = nc
    D = {}
    c.D = D

    def din(name, shape, dt=F32):
        D[name] = nc.dram_tensor(name, list(shape), dt, kind="ExternalInput").ap()

    din("xp", [SEQ, D_MODEL])
    din("cT", [128, 8])
    din("ada_w", [D_MODEL, 6 * D_MODEL])
    din("ada_b", [1, 6 * D_MODEL])
    din("pln", [4, 128, D_MODEL])
    din("w_r", [D_MODEL, NEXP])
    din("b_r", [1, NEXP])
    import os
    ph = os.environ.get("KPH", "mod,mix,post,moe").split(",")
    if "moe" in ph:
        din("w_gu", [NEXP, D_MODEL, 2 * D_MODEL])
        din("b_guT", [128, NEXP, 16])
        din("w_down", [NEXP, D_MODEL, D_MODEL])
        din("b_down", [NEXP, D_MODEL])
    if m == 1:
        din("w_qkv", [D_MODEL, 2048])
        din("g12", [128, 12, 128])
        din("rope", [32, 128, 2, 128])
        din("w_o", [D_MODEL, D_MODEL])
    if m == 2:
        din("s5_are", [2, 128, 64, 64])
        din("s5_aim", [2, 128, 64, 64])
        din("s5_ldt", [2, 128, 64])
        din("s5_bre", [2, 128, 64, 64])
        din("s5_bim", [2, 128, 64, 64])
        din("s5_c1", [2, 128, 64, 16])
        din("s5_c2", [2, 128, 64, 16])
        din("s5_colp", [2, 128, 3, 64])
        din("s5_maskB", [128, 64])
        din("s5_maskJ", [128, 64, 8])
        din("s5_sgn", [128, 2])
        din("s5_d", [128, 8])
        din("w_glu", [D_MODEL, D_MODEL])
        din("w_so", [D_MODEL, D_MODEL])
    if m == 3:
        din("w_in", [D_MODEL, 5, D_MODEL])
        din("lbT", [128, 4, 8])
        din("cmask", [128, HALF])
        din("tri", [64, 2, 64])
        din("hnorm", [64, 8, 128])
        din("w_ho", [D_MODEL, D_MODEL])
    if m == 0:
        din("dftL", [4, 8, 128, 4, 2, 512], BF16)
        din("dftC", [128, 2, 2, 256], BF16)
        din("fnet_w", [D_MODEL, D_MODEL])
        din("fnet_b", [1, D_MODEL])
    D["out"] = nc.dram_tensor("out", [HALF, D_MODEL], F32, kind="ExternalOutput").ap()
    c.modd = nc.dram_tensor("modd", [128, 6 * D_MODEL], F32).ap()
    if "moe" in ph:
        c.x1buf = nc.dram_tensor("x1buf", [HALF, D_MODEL], F32).ap()
    else:
        c.x1buf = nc.dram_tensor("x1buf", [HALF, D_MODEL], F32, kind="ExternalOutput").ap()
    c.h2T = nc.dram_tensor("h2T", [128, 8, HALF], BF16).ap()
    c.gbuf = nc.dram_tensor("gbuf", [16, 128, NEXP], F32).ap()
    ymix = nc.dram_tensor("ymix", [HALF, D_MODEL], F32).ap()
    c.p = Prog(nc)
    setup_consts(c)
    if "mod" in ph:
        phase_mod(c)
    if "mix" in ph:
        if m == 0:
            phase_fnet(c, ymix)
        elif m == 1:
            phase_attn(c, ymix)
        elif m == 2:
            phase_s5(c, ymix)
        elif m == 3:
            phase_hgrn(c, ymix, 3)
    if "post" in ph:
        phase_post(c, ymix)
    if "moe" in ph:
        phase_moe(c)
    c.p.finish()
    c.n_ops = c.p.total_ops
    return nc, c


_PROGS = {}


def get_prog(m):
    if m not in _PROGS:
        _PROGS[m] = build_layer(m)
    return _PROGS[m]


def bf16(a):
    return np.asarray(a, dtype=np.float32).astype(ml_dtypes.bfloat16)


def present(xb, hf):
    return np.ascontiguousarray(xb if hf == 0 else xb[::-1])


def unpresent_idx(hf):
    j = np.arange(HALF)
    return j if hf == 0 else (SEQ - 1 - j)


def fnet_tables(hf):
    j = np.arange(SEQ, dtype=np.int64)
    l_in = j if hf == 0 else SEQ - 1 - j
    l_out = l_in[:HALF]
    ph = (l_in[:, None] * l_out[None, :]) % SEQ
    ang = (2.0 * np.pi / SEQ) * ph.astype(np.float64)
    tab = np.stack([np.cos(ang), np.sin(ang)], axis=1)
    tab = tab.reshape(8, 4, 128, 2, 4, 512).transpose(4, 0, 2, 1, 3, 5)
    return bf16(np.ascontiguousarray(tab))


def fnet_ctab():
    k = np.arange(256, dtype=np.int64)
    ang = (2.0 * np.pi / 256) * ((k[:, None] * k[None, :]) % 256).astype(np.float64)
    t = np.stack([np.cos(ang), -np.sin(ang)], axis=1)
    t = t.reshape(2, 128, 2, 256).transpose(1, 0, 2, 3)
    return bf16(np.ascontiguousarray(t))


def common_inputs(inp, li, b):
    f = np.float32
    d = {}
    d["cT"] = np.ascontiguousarray(inp["c"][b].reshape(8, 128).T).astype(f)
    d["ada_w"] = inp["ada_w"][li]
    d["ada_b"] = inp["ada_b"][li][None, :]
    pl = np.stack([inp["post_ln_g"][li, 0], inp["post_ln_b"][li, 0], inp["post_ln_g"][li, 1], inp["post_ln_b"][li, 1]])
    d["pln"] = np.ascontiguousarray(np.broadcast_to(pl[:, None, :], (4, 128, D_MODEL))).astype(f)
    d["w_r"] = inp["moe_w_router"][li]
    d["b_r"] = inp["moe_b_router"][li][None, :]
    d["w_gu"] = inp["moe_w_gu"][li]
    d["b_guT"] = np.ascontiguousarray(inp["moe_b_gu"][li].reshape(NEXP, 16, 128).transpose(2, 0, 1))
    d["w_down"] = inp["moe_w_down"][li]
    d["b_down"] = inp["moe_b_down"][li]
    return d


def run_layer(li, x, inp):
    m = li % 4
    j = li // 4
    nc, c = get_prog(m)
    in_maps = []
    shared = {}
    for core in range(8):
        b, hf = core // 2, core % 2
        if b not in shared:
            shared[b] = common_inputs(inp, li, b)
        d = dict(shared[b])
        d["xp"] = present(x[b], hf)
        if m == 0:
            d["dftL"] = fnet_tables(hf)
            d["dftC"] = fnet_ctab()
            d["fnet_w"] = inp["fnet_w_out"][j]
            d["fnet_b"] = inp["fnet_b_out"][j][None, :]
        if m == 2:
            d.update(s5_inputs(inp, j, hf))
        if m == 3:
            w = inp["hg_w_in"][j].reshape(D_MODEL, 5, D_MODEL)
            if hf == 1:
                w = w[:, [0, 2, 1, 3, 4], :]
            d["w_in"] = np.ascontiguousarray(w)
            d["lbT"] = np.ascontiguousarray(inp["hg_lb"].reshape(4, 8, 128).transpose(2, 0, 1))
            cmk = np.ones((128, HALF), np.float32)
            cmk[:, ::64] = 0.0
            d["cmask"] = cmk
            sidx = np.arange(64)
            d["tri"] = np.stack([(sidx[:, None] <= sidx[None, :]), (sidx[:, None] >= sidx[None, :])], axis=1).astype(np.float32)
            d["hnorm"] = np.ascontiguousarray(np.broadcast_to(inp["hg_norm"][j].reshape(1, 8, 128), (64, 8, 128))).astype(np.float32)
            d["w_ho"] = inp["hg_w_out"][j]
        if m == 1:
            d["w_qkv"] = inp["attn_w_qkv"][j]
            g = np.concatenate([np.tile(inp["attn_q_norm"][j][None, :], (8, 1)), np.tile(inp["attn_k_norm"][j][None, :], (4, 1))])
            d["g12"] = np.ascontiguousarray(np.broadcast_to(g[None], (128, 12, 128))).astype(np.float32)
            d["rope"] = rope_tables(hf)
            d["w_o"] = inp["attn_w_out"][j]
        in_maps.append({k: v for k, v in d.items() if k in c.D})
    res = run_bass_kernel_spmd(nc, in_maps, core_ids=list(range(8)))
    out = np.empty_like(x)
    key = "out" if "w_gu" in c.D else "x1buf"
    for core in range(8):
        b, hf = core // 2, core % 2
        out[b, unpresent_idx(hf)] = res.results[core][key]
    return out


def phase_attn(c, ymix):
    p, D = c.p, c.D
    SCALE = 128.0 ** -0.5
    with p.scope():
        kT = p.sbuf("kT", [128, 4, SEQ], BF16)
        qT = p.sbuf("qT", [128, 8, HALF], BF16)
        Va = p.sbuf("Va", [128, 32, 4, 129], BF16)
        with p.scope():
            mods = p.sbuf("modsQ", [128, 2, 1024], F32)
            p.dma("sp", mods[:, 0, :], c.modd[:, 0:1024], w=["mods"])
            p.dma("sp", mods[:, 1, :], c.modd[:, 1024:2048], w=["mods"])
            wq = p.sbuf("wqkv", [128, 8, 2048], BF16)
            p.dma("pool", wq[:], D["w_qkv"].rearrange("(c p) n -> p c n", p=128), w=["wq"])
            g12 = p.sbuf("g12", [128, 12, 128], F32)
            p.dma("act", g12[:], D["g12"][:, :, :], w=["g12"])
            p.memset("pool", Va[:, :, :, 128:129], 1.0, w=["Va1"])
            xt = Rot(p, "xtQ", [128, 1024], F32, 2)
            ln = LN(p, "lnQ")
            hb = Rot(p, "hbQ", [128, 1024], BF16, 2)
            hTt = Rot(p, "hTtQ", [128, 8, 128], BF16, 2)
            pT = Rot(p, "pTQ", [128, 4, 128], F32, 3, psum=True)
            pq = Rot(p, "pqQ", [128, 4, 128], F32, 4, psum=True)
            qk = Rot(p, "qkQ", [128, 12, 128], F32, 2)
            sq = p.sbuf("sqQ", [128, 12, 128], F32)
            xr = p.sbuf("xrQ", [128, 12, 128], F32)
            rq = Rot(p, "rqQ", [128, 12, 128], BF16, 2)
            ss = Rot(p, "ssQ", [128, 12], F32, 2)
            rope = Rot(p, "ropeQ", [128, 2, 128], F32, 2)
            for t in range(32):
                own = t < 16
                x, xk = xt.next()
                p.dma("sp", x[:], D["xp"][t * 128:(t + 1) * 128, :], w=[xk])
                h, hk = hb.next()
                ln(x[:], xk, h[:], hk, mods[:, 1, :], mods[:, 0, :], ["mods"])
                hT_, hTk = hTt.next()
                for q in range(2):
                    ps, psk = pT.next()
                    for k in range(4):
                        p.mm(ps[:, k, :], h[:, (q * 4 + k) * 128:(q * 4 + k + 1) * 128], c.identb[:], r=[hk], w=[(psk, k)])
                    p.copy("act" if q == 0 else "dve", hT_[:, q * 4:(q + 1) * 4, :], ps[:],
                           r=[(psk, k) for k in range(4)], w=[(hTk, q)])
                hTk2 = [(hTk, 0), (hTk, 1)]
                qk_, qkk = qk.next()
                for bi in ([0, 1] if own else []) + [2, 3]:
                    pb, pbk = pq.next()
                    pbf = pb[:].rearrange("p h d -> p (h d)")
                    for k in range(8):
                        p.mm(pbf, hT_[:, k, :], wq[:, k, bi * 512:(bi + 1) * 512], start=(k == 0), stop=(k == 7),
                             r=hTk2 + ["wq"], w=[pbk])
                    if bi == 3:
                        p.copy("act", Va[:, t, :, 0:128], pb[:], r=[pbk], w=[("Va", t)])
                    else:
                        p.copy("dve", qk_[:, bi * 4:(bi + 1) * 4, :], pb[:], r=[pbk], w=[(qkk, bi)])
                h0 = 0 if own else 8
                H = 12 - h0
                v = qk_[:, h0:12, :]
                vk = [(qkk, b) for b in (0, 1, 2) if own or b == 2]
                p.tt("pool", sq[:, h0:12, :], v, v, ALU.mult, r=vk, w=["sq"])
                s_, sk = ss.next()
                p.op("dve", (lambda e, s_=s_, h0=h0: e.reduce_sum(out=s_[:, h0:12], in_=sq[:, h0:12, :], axis=AX.X)),
                     r=["sq"], w=[sk])
                p.ts("dve", s_[:, h0:12], s_[:, h0:12], 1.0 / 128.0, ALU.mult, RMS_EPS, ALU.add, r=[sk], w=[sk])
                p.op("act", (lambda e, s_=s_, h0=h0: e.sqrt(out=s_[:, h0:12], in_=s_[:, h0:12])), r=[sk], w=[sk])
                p.op("dve", (lambda e, s_=s_, h0=h0: e.reciprocal(out=s_[:, h0:12], in_=s_[:, h0:12])), r=[sk], w=[sk])
                p.tt("dve", v, v, s_[:, h0:12].unsqueeze(2).to_broadcast([128, H, 128]), ALU.mult, r=vk + [sk], w=vk)
                p.tt("pool", v, v, g12[:, h0:12, :], ALU.mult, r=vk + ["g12"], w=vk)
                rp, rpk = rope.next()
                p.dma("act", rp[:], D["rope"][t], w=[rpk])
                v4 = v.rearrange("p h (b s e) -> p (h b) s e", b=2, s=2, e=32)
                x4 = xr[:, h0:12, :].rearrange("p h (b s e) -> p (h b) s e", b=2, s=2, e=32)
                p.copy("pool", x4[:, :, 0, :], v4[:, :, 1, :], r=vk, w=["xr0"])
                p.copy("pool", x4[:, :, 1, :], v4[:, :, 0, :], r=vk, w=["xr1"])
                cosb = rp[:, 0, :].unsqueeze(1).to_broadcast([128, H, 128])
                sinb = rp[:, 1, :].unsqueeze(1).to_broadcast([128, H, 128])
                p.tt("dve", v, v, cosb, ALU.mult, r=vk + [rpk], w=vk)
                p.tt("pool", xr[:, h0:12, :], xr[:, h0:12, :], sinb, ALU.mult, r=["xr0", "xr1", rpk], w=["xr0", "xr1"])
                r_, rk = rq.next()
                p.tt("dve", r_[:, h0:12, :], v, xr[:, h0:12, :], ALU.add, r=vk + ["xr0", "xr1"], w=[rk])
                for gi, g0 in enumerate(range(h0, 12, 4)):
                    ps, psk = pT.next()
                    for i in range(4):
                        p.mm(ps[:, i, :], r_[:, g0 + i, :], c.identb[:], r=[rk], w=[(psk, i)])
                    if g0 < 8:
                        dst = qT[:, g0:g0 + 4, t * 128:(t + 1) * 128]
                        dk = ("qT", t, g0)
                    else:
                        dst = kT[:, 0:4, t * 128:(t + 1) * 128]
                        dk = ("kT", t)
                    p.copy("act" if gi % 2 == 0 else "dve", dst, ps[:], r=[(psk, i) for i in range(4)], w=[dk])
        Ot = p.sbuf("Otok", [128, 16, 8, 128], BF16)
        with p.scope():
            pS = Rot(p, "pS", [128, 512], F32, 2, psum=True)
            pO = Rot(p, "pO", [128, 512], F32, 4, psum=True)
            PT = Rot(p, "PT", [128, 512], BF16, 3)
            rc = Rot(p, "rc", [128, 1], F32, 4)
            for head in range(8):
                kvh = head // 2
                for qc in range(4):
                    accs = [pO.next() for _ in range(4)]
                    for st in range(32):
                        s_, sk = pS.next()
                        p.mm(s_[:], kT[:, kvh, st * 128:(st + 1) * 128], qT[:, head, qc * 512:(qc + 1) * 512], w=[sk])
                        pt_, ptk = PT.next()
                        p.act(pt_[:], s_[:], ACT.Exp, scale=SCALE, r=[sk], w=[ptk])
                        for qs in range(4):
                            o_, ok = accs[qs]
                            p.mm(o_[:, 0:129], pt_[:, qs * 128:(qs + 1) * 128], Va[:, st, kvh, :],
                                 start=(st == 0), stop=(st == 31), r=[ptk], w=[ok])
                    for qs in range(4):
                        o_, ok = accs[qs]
                        r_, rk = rc.next()
                        p.op("dve", (lambda e, r_=r_, o_=o_: e.reciprocal(out=r_[:], in_=o_[:, 128:129])), r=[ok], w=[rk])
                        p.ts("dve", Ot[:, qc * 4 + qs, head, :], o_[:, 0:128], r_[:, 0:1], ALU.mult, r=[ok, rk],
                             w=[("Ot", qc * 4 + qs, head)])
        with p.scope():
            wo = p.sbuf("woQ", [128, 8, 1024], BF16)
            p.dma("pool", wo[:], D["w_o"].rearrange("(c p) n -> p c n", p=128), w=["wo"])
            pT = Rot(p, "pTo", [128, 4, 128], F32, 2, psum=True)
            pY = Rot(p, "pYo", [128, 512], F32, 2, psum=True)
            oT = Rot(p, "oTo", [128, 8, 128], BF16, 2)
            yo = Rot(p, "yoQ", [128, 1024], F32, 2)
            for qt in range(16):
                o_, otk = oT.next()
                for g in range(2):
                    ps, psk = pT.next()
                    for i in range(4):
                        p.mm(ps[:, i, :], Ot[:, qt, g * 4 + i, :], c.identb[:], w=[(psk, i)])
                    p.copy("act" if g == 0 else "dve", o_[:, g * 4:(g + 1) * 4, :], ps[:],
                           r=[(psk, i) for i in range(4)], w=[(otk, g)])
                y_, yk = yo.next()
                for hh in range(2):
                    py, pyk = pY.next()
                    for hd in range(8):
                        p.mm(py[:], o_[:, hd, :], wo[:, hd, hh * 512:(hh + 1) * 512], start=(hd == 0), stop=(hd == 7),
                             r=[(otk, 0), (otk, 1), "wo"], w=[pyk])
                    p.copy("act" if hh == 0 else "dve", y_[:, hh * 512:(hh + 1) * 512], py[:], r=[pyk], w=[(yk, hh)])
                p.dma("sp", ymix[qt * 128:(qt + 1) * 128, :], y_[:], r=[(yk, 0), (yk, 1)], w=[("ymix", qt)])


def rope_tables(hf):
    j = np.arange(SEQ)
    l = j if hf == 0 else SEQ - 1 - j
    row = (l // 64).astype(np.float32)
    col = (l % 64).astype(np.float32)
    inv = (np.float32(10000.0) ** (-np.arange(0, 64, 2, dtype=np.float32) / np.float32(64))).astype(np.float32)
    ar = (row[:, None] * inv[None, :]).astype(np.float32)
    ac = (col[:, None] * inv[None, :]).astype(np.float32)
    cr, sr, cc, sc = np.cos(ar), np.sin(ar), np.cos(ac), np.sin(ac)
    cos = np.concatenate([cr, cr, cc, cc], axis=1)
    sin = np.concatenate([-sr, sr, -sc, sc], axis=1)
    t = np.stack([cos, sin], axis=1).astype(np.float32)
    return np.ascontiguousarray(t.reshape(32, 128, 2, 128))


def phase_hgrn(c, ymix, li):
    p, D = c.p, c.D
    CH = 64
    with p.scope():
        hTd = c.nc.dram_tensor("hTdH" + getattr(c, "lsuffix", ""), [128, 8, SEQ], BF16).ap()
        oTall = p.sbuf("oTall", [128, 8, HALF], BF16)
        lbt = p.sbuf("lbt", [128, 4, 8], F32)
        lb = p.sbuf("lb", [128, 8], F32)
        oml = p.sbuf("oml", [128, 8], F32)
        noml = p.sbuf("noml", [128, 8], F32)
        den = p.sbuf("lbden", [128, 8], F32)
        cm = p.sbuf("cmaskH", [128, HALF], F32)
        tri = p.sbuf("triH", [64, 2, 64], F32)
        hn = p.sbuf("hnH", [64, 8, 128], F32)
        p.dma("sp", lbt[:], D["lbT"][:, :, :], w=["lbt"])
        p.dma("sp", cm[:], D["cmask"][:, :], w=["cm"])
        p.dma("sp", tri[:], D["tri"][:, :, :], w=["tri"])
        p.dma("sp", hn[:], D["hnorm"][:, :, :], w=["hn"])
        p.act(lbt[:], lbt[:], ACT.Exp, r=["lbt"], w=["lbt"])
        p.copy("dve", den[:], lbt[:, 0, :], r=["lbt"], w=["den"])
        for j in range(1, 4):
            p.tt("dve", den[:], den[:], lbt[:, j, :], ALU.add, r=["den", "lbt"], w=["den"])
        p.memset("dve", lb[:], 0.0, w=["lb"])
        for j in range(1, li + 1):
            p.tt("dve", lb[:], lb[:], lbt[:, j, :], ALU.add, r=["lb", "lbt"], w=["lb"])
        p.op("dve", (lambda e: e.reciprocal(out=den[:], in_=den[:])), r=["den"], w=["den"])
        p.tt("dve", lb[:], lb[:], den[:], ALU.mult, r=["lb", "den"], w=["lb"])
        p.ts("dve", oml[:], lb[:], -1.0, ALU.mult, 1.0, ALU.add, r=["lb"], w=["oml"])
        p.ts("dve", noml[:], oml[:], -1.0, ALU.mult, r=["oml"], w=["noml"])
        with p.scope():
            mods = p.sbuf("modsH", [128, 2, 1024], F32)
            p.dma("sp", mods[:, 0, :], c.modd[:, 0:1024], w=["mods"])
            p.dma("sp", mods[:, 1, :], c.modd[:, 1024:2048], w=["mods"])
            xt = Rot(p, "xtH", [128, 1024], F32, 2)
            ln = LN(p, "lnH")
            hb = Rot(p, "hbH", [128, 1024], BF16, 2)
            pT = Rot(p, "pTH", [128, 4, 128], F32, 4, psum=True)
            hTt = Rot(p, "hTtH", [128, 8, 128], BF16, 2)
            for t in range(32):
                x, xk = xt.next()
                p.dma("sp" if t % 2 == 0 else "act", x[:], D["xp"][t * 128:(t + 1) * 128, :], w=[xk])
                h, hk = hb.next()
                ln(x[:], xk, h[:], hk, mods[:, 1, :], mods[:, 0, :], ["mods"])
                hT_, hTk = hTt.next()
                for q in range(2):
                    ps, psk = pT.next()
                    for k in range(4):
                        p.mm(ps[:, k, :], h[:, (q * 4 + k) * 128:(q * 4 + k + 1) * 128], c.identb[:], r=[hk], w=[(psk, k)])
                    p.copy("act" if q == 0 else "dve", hT_[:, q * 4:(q + 1) * 4, :], ps[:],
                           r=[(psk, k) for k in range(4)], w=[(hTk, q)])
                p.dma("sp", hTd[:, :, t * 128:(t + 1) * 128], hT_[:], r=[(hTk, 0), (hTk, 1)], w=[("hTd", t)])
        p.flush()
        with p.scope():
            wh = Rot(p, "whH", [128, 8, 5, 128], BF16, 1)
            hs = p.sbuf("hsH", [128, 8, HALF], BF16)
            qf = p.sbuf("qfH", [128, HALF], F32)
            t1 = p.sbuf("t1H", [128, HALF], F32)
            kf = p.sbuf("kfH", [128, HALF], F32)
            bb = p.sbuf("bbH", [128, HALF], F32)
            kl = p.sbuf("klH", [128, HALF], BF16)
            qd = p.sbuf("qdH", [128, HALF], BF16)
            kk = p.sbuf("kkH", [128, HALF], BF16)
            dec = p.sbuf("decH", [128, 32], F32)
            vtok = p.sbuf("vtokH", [64, 32, 128], BF16)
            oA = p.sbuf("oAH", [64, 32, 128], F32)
            sgt = p.sbuf("sgtH", [64, 32, 128], BF16)
            og = p.sbuf("ogH", [64, 32, 128], BF16)
            ssn = p.sbuf("ssnH", [64, 32], F32)
            S = p.sbuf("SH", [128, 128], F32)
            Sb = p.sbuf("SbH", [128, 128], BF16)
            klt = Rot(p, "kltH", [64, 128], BF16, 2)
            attT = Rot(p, "attTH", [64, 64], BF16, 2)
            pbig = Rot(p, "pbigH", [128, 512], F32, 2, psum=True)
            pkv = Rot(p, "pkvH", [128, 512], F32, 2, psum=True)
            pa = Rot(p, "paH", [128, 512], F32, 2, psum=True)
            po = Rot(p, "poH", [128, 512], F32, 2, psum=True)
            for h in range(8):
                w_, wk = wh.next()
                fmap = [0, 2, 1, 3, 4] if getattr(c, "vhf", 0) else [0, 1, 2, 3, 4]
                for f_ in range(5):
                    p.dma("pool", w_[:, :, f_, :], D["w_in"][:, fmap[f_], h * 128:(h + 1) * 128].rearrange("(c p) n -> p c n", p=128), w=[wk])

                def proj_fm(fi, dst, func, dk_):
                    for blk in range(4):
                        ps, psk = pbig.next()
                        for k in range(8):
                            p.mm(ps[:], w_[:, k, fi, :], hs[:, k, blk * 512:(blk + 1) * 512],
                                 start=(k == 0), stop=(k == 7), r=[wk, "hs"], w=[psk])
                        p.act(dst[:, blk * 512:(blk + 1) * 512], ps[:], func, r=[psk], w=[(dk_, blk)])
                    return [(dk_, b) for b in range(4)]

                def proj_tm(fi, nchunks, dst, dk_, func):
                    for n0 in range(0, nchunks, 4):
                        ps, psk = pbig.next()
                        for i in range(4):
                            n = n0 + i
                            for k in range(8):
                                p.mm(ps[0:64, i * 128:(i + 1) * 128], hs[:, k, n * CH:(n + 1) * CH], w_[:, k, fi, :],
                                     start=(k == 0), stop=(k == 7), r=[wk, "hs"], w=[(psk, i)])
                        src = ps[0:64, :].rearrange("p (i d) -> p i d", i=4)
                        p.act(dst[:, n0:n0 + 4, :], src, func, r=[(psk, i) for i in range(4)], w=[(dk_, n0)])

                qk_ = [("qf", b_) for b_ in range(4)]
                vkeys = [("vt", n0) for n0 in range(0, 32, 4)]
                p.memset("dve", S[:], 0.0, w=["S"])
                p.memset("pool", Sb[:], 0.0, w=["Sb"])
                for seg, (fi, tok0, outputs, rev) in enumerate([(1, 0, True, False), (2, HALF, False, True), (2, 0, True, True)]):
                    if seg == 1:
                        p.memset("dve", S[:], 0.0, w=["S"])
                        p.memset("pool", Sb[:], 0.0, w=["Sb"])
                    p.dma("sp", hs[:], hTd[:, :, tok0:tok0 + HALF], w=["hs"])
                    if seg == 0:
                        proj_fm(0, qf, ACT.Silu, "qf")
                        proj_tm(4, 32, sgt, "sgt", ACT.Silu)
                    proj_tm(3, 32, vtok, "vt", ACT.Copy)
                    tk = proj_fm(fi, t1, ACT.Sigmoid, "t1")
                    p.ts("pool", kf[:], t1[:], noml[:, h:h + 1], ALU.mult, oml[:, h:h + 1], ALU.add, r=tk + ["noml", "oml"], w=["kf"])
                    p.ts("dve", t1[:], t1[:], oml[:, h:h + 1], ALU.mult, lb[:, h:h + 1], ALU.add, r=tk + ["oml", "lb"], w=tk)
                    p.act(t1[:], t1[:], ACT.Ln, r=tk, w=tk)
                    if rev:
                        p.op("dve", (lambda e: e.tensor_tensor_scan(out=bb[:, ::-1], data0=cm[:], data1=t1[:, ::-1], initial=0.0,
                                                                     op0=ALU.mult, op1=ALU.add)), r=tk + ["cm"], w=["bb"])
                    else:
                        p.op("dve", (lambda e: e.tensor_tensor_scan(out=bb[:], data0=cm[:], data1=t1[:], initial=0.0,
                                                                     op0=ALU.mult, op1=ALU.add)), r=tk + ["cm"], w=["bb"])
                    b3 = bb[:].rearrange("p (n c) -> p n c", c=CH)
                    bl = b3[:, :, 0:1] if rev else b3[:, :, CH - 1:CH]
                    t3 = t1[:].rearrange("p (n c) -> p n c", c=CH)
                    p.tt("pool", t3, bl.to_broadcast([128, 32, CH]), b3, ALU.subtract, r=["bb"] + tk, w=tk)
                    p.act(t1[:], t1[:], ACT.Exp, r=tk, w=tk)
                    p.tt("dve", kl[:], kf[:], t1[:], ALU.mult, r=["kf"] + tk, w=["kl"])
                    p.act(dec[:].unsqueeze(2), bl, ACT.Exp, r=["bb"], w=["dec"])
                    if outputs:
                        p.act(t1[:], bb[:], ACT.Exp, r=["bb", "kl"] + tk, w=tk)
                        p.tt("pool", qd[:], qf[:], t1[:], ALU.mult, r=qk_ + tk, w=["qd"])
                        p.act(t1[:], bb[:], ACT.Exp, scale=-1.0, r=["bb", "qd"] + tk, w=tk)
                        p.tt("dve", kk[:], kf[:], t1[:], ALU.mult, r=["kf"] + tk, w=["kk"])
                    order = range(31, -1, -1) if rev else range(32)
                    vbase = 0
                    for n in order:
                        cs = slice(n * CH, (n + 1) * CH)
                        ps, psk = pa.next()
                        p.mm(ps[0:64, 0:128], kl[:, cs], c.identb[:], r=["kl"], w=[psk])
                        kt_, ktk = klt.next()
                        p.copy("act", kt_[:], ps[0:64, 0:128], r=[psk], w=[ktk])
                        pv, pvk = pkv.next()
                        p.mm(pv[:, 0:128], kt_[:], vtok[:, vbase + n, :], r=[ktk, vkeys[n // 4]], w=[pvk])
                        if outputs:
                            ps2, ps2k = pa.next()
                            p.mm(ps2[0:64, 0:64], kk[:, cs], qd[:, cs], r=["kk", "qd"], w=[ps2k])
                            at_, atk = attT.next()
                            p.tt("dve", at_[:], ps2[0:64, 0:64], tri[:, 1 if rev else 0, :], ALU.mult, r=[ps2k, "tri"], w=[atk])
                            po_, pok = po.next()
                            p.mm(po_[0:64, 0:128], at_[:], vtok[:, vbase + n, :], start=True, stop=False,
                                 r=[atk, vkeys[n // 4]], w=[pok])
                            p.mm(po_[0:64, 0:128], qd[:, cs], Sb[:], start=False, stop=True, r=["qd", "Sb"], w=[pok])
                            if seg == 0:
                                p.copy("act", oA[:, n, :], po_[0:64, 0:128], r=[pok], w=[("oA", n)])
                            else:
                                p.tt("dve", oA[:, n, :], oA[:, n, :], po_[0:64, 0:128], ALU.add, r=[pok, ("oA", n)], w=[("oA", n)])
                        p.stt("dve", S[:], S[:], dec[:, n:n + 1], pv[:, 0:128], ALU.mult, ALU.add, r=["S", "dec", pvk], w=["S"])
                        p.copy("pool", Sb[:], S[:], r=["S"], w=["Sb"])
                oAk = [("oA", n) for n in range(32)]
                p.tt("pool", og[:], oA[:], oA[:], ALU.mult, r=oAk, w=["og"])
                p.op("dve", (lambda e: e.reduce_sum(out=ssn[:], in_=og[:], axis=AX.X)), r=["og"], w=["ssn"])
                p.ts("dve", ssn[:], ssn[:], 1.0 / 128.0, ALU.mult, RMS_EPS, ALU.add, r=["ssn"], w=["ssn"])
                p.op("act", (lambda e: e.sqrt(out=ssn[:], in_=ssn[:])), r=["ssn"], w=["ssn"])
                p.op("dve", (lambda e: e.reciprocal(out=ssn[:], in_=ssn[:])), r=["ssn"], w=["ssn"])
                p.tt("dve", oA[:], oA[:], ssn[:].unsqueeze(2).to_broadcast([64, 32, 128]), ALU.mult, r=oAk + ["ssn"], w=oAk)
                p.tt("pool", oA[:], oA[:], hn[:, h, :].unsqueeze(1).to_broadcast([64, 32, 128]), ALU.mult, r=oAk + ["hn"], w=oAk)
                p.tt("dve", og[:], oA[:], sgt[:], ALU.mult, r=oAk + [("sgt", n0) for n0 in range(0, 32, 4)], w=["og"])
                for n0 in range(0, 32, 8):
                    ps, psk = pbig.next()
                    for i in range(8):
                        p.mm(ps[:, i * 64:(i + 1) * 64], og[:, n0 + i, :], c.identb[0:64, 0:64], r=["og"], w=[(psk, i)])
                    p.copy("act", oTall[:, h, n0 * 64:(n0 + 8) * 64], ps[:], r=[(psk, i) for i in range(8)], w=[("oTall", h, n0)])
        with p.scope():
            wo = p.sbuf("woH", [128, 8, 1024], BF16)
            p.dma("pool", wo[:], D["w_ho"].rearrange("(c p) n -> p c n", p=128), w=["wo"])
            pY = Rot(p, "pYH", [128, 512], F32, 2, psum=True)
            yo = Rot(p, "yoH", [128, 1024], F32, 2)
            for qt in range(16):
                y_, yk = yo.next()
                for hh in range(2):
                    py, pyk = pY.next()
                    for hd in range(8):
                        p.mm(py[:], oTall[:, hd, qt * 128:(qt + 1) * 128], wo[:, hd, hh * 512:(hh + 1) * 512],
                             start=(hd == 0), stop=(hd == 7), r=["wo"], w=[pyk])
                    p.copy("act" if hh == 0 else "dve", y_[:, hh * 512:(hh + 1) * 512], py[:], r=[pyk], w=[(yk, hh)])
                p.dma("sp", ymix[qt * 128:(qt + 1) * 128, :], y_[:], r=[(yk, 0), (yk, 1)], w=[("ymix", qt)])


def _sincos(p, th, sn, cs, tmp, t2, keys_r, kout):
    PI = math.pi
    for which, dst in ((0, sn), (1, cs)):
        if which == 0:
            p.copy("dve", t2, th, r=keys_r, w=[kout + "_t2"])
        else:
            p.ts("dve", t2, th, PI / 2, ALU.add, r=keys_r, w=[kout + "_t2"])
        p.copy("dve", dst, t2, r=[kout + "_t2"], w=[kout + str(which)])
        for i in range(1, 9):
            p.ts("dve", tmp, t2, (2 * i - 1) * PI, ALU.is_gt, 2 * PI, ALU.mult, r=[kout + "_t2"], w=[kout + "_tmp"])
            p.tt("dve", dst, dst, tmp, ALU.subtract, r=[kout + str(which), kout + "_tmp"], w=[kout + str(which)])
        p.act(dst, dst, ACT.Sin, r=[kout + str(which)], w=[kout + str(which)])


def phase_s5(c, ymix):
    p, D = c.p, c.D
    with p.scope():
        hTd = c.nc.dram_tensor("hTdS" + getattr(c, "lsuffix", ""), [128, 8, SEQ], BF16).ap()
        ygT = p.sbuf("ygT", [128, 8, HALF], BF16)
        with p.scope():
            mods = p.sbuf("modsS", [128, 2, 1024], F32)
            p.dma("sp", mods[:, 0, :], c.modd[:, 0:1024], w=["mods"])
            p.dma("sp", mods[:, 1, :], c.modd[:, 1024:2048], w=["mods"])
            xt = Rot(p, "xtS", [128, 1024], F32, 2)
            ln = LN(p, "lnS")
            hb = Rot(p, "hbS", [128, 1024], BF16, 2)
            pT = Rot(p, "pTS", [128, 4, 128], F32, 4, psum=True)
            hTt = Rot(p, "hTtS", [128, 8, 128], BF16, 2)
            for t in range(32):
                x, xk = xt.next()
                p.dma("sp" if t % 2 == 0 else "act", x[:], D["xp"][t * 128:(t + 1) * 128, :], w=[xk])
                h, hk = hb.next()
                ln(x[:], xk, h[:], hk, mods[:, 1, :], mods[:, 0, :], ["mods"])
                hT_, hTk = hTt.next()
                for q in range(2):
                    ps, psk = pT.next()
                    for k in range(4):
                        p.mm(ps[:, k, :], h[:, (q * 4 + k) * 128:(q * 4 + k + 1) * 128], c.identb[:], r=[hk], w=[(psk, k)])
                    p.copy("act" if q == 0 else "dve", hT_[:, q * 4:(q + 1) * 4, :], ps[:],
                           r=[(psk, k) for k in range(4)], w=[(hTk, q)])
                p.dma("sp", hTd[:, :, t * 128:(t + 1) * 128], hT_[:], r=[(hTk, 0), (hTk, 1)], w=[("hTd", t)])
        with p.scope():
            colp = p.sbuf("colp", [128, 2, 3, 64], F32)
            vsw = getattr(c, "vhf", 0)
            for dd_ in range(2):
                p.dma("sp", colp[:, dd_, :, :], D["s5_colp"][dd_ ^ vsw], w=["colp"])
            rc = p.sbuf("rcS", [128, 2, 64], F32)
            thc = p.sbuf("thcS", [128, 2, 64], F32)
            DC = p.sbuf("DCS", [128, 2, 12, 64], F32)
            DS = p.sbuf("DSS", [128, 2, 12, 64], F32)
            NDS = p.sbuf("NDSS", [128, 2, 12, 64], F32)
            c1 = p.sbuf("c1S", [128, 2, 64], F32)
            c2 = p.sbuf("c2S", [128, 2, 64], F32)
            c3 = p.sbuf("c3S", [128, 2, 64], F32)
            c4 = p.sbuf("c4S", [128, 2, 64], F32)
            c5 = p.sbuf("c5S", [128, 2, 64], F32)
            c6 = p.sbuf("c6S", [128, 2, 64], F32)
            p.act(c1[:], colp[:, :, 2, :], ACT.Exp, r=["colp"], w=["c1"])
            p.tt("dve", rc[:], colp[:, :, 0, :], c1[:], ALU.mult, r=["colp", "c1"], w=["rc"])
            p.act(rc[:], rc[:], ACT.Exp, r=["rc"], w=["rc"])
            p.tt("dve", thc[:], colp[:, :, 1, :], c1[:], ALU.mult, r=["colp", "c1"], w=["thc"])
            _sincos(p, thc[:], c2[:], c3[:], c5[:], c6[:], ["thc"], "scC")
            p.copy("dve", DS[:, :, 0, :], c2[:], r=["scC0"], w=[("DS", 0)])
            p.copy("dve", DC[:, :, 0, :], c3[:], r=["scC1"], w=[("DC", 0)])
            for k in range(1, 12):
                cp_, sp_ = DC[:, :, k - 1, :], DS[:, :, k - 1, :]
                p.tt("dve", c1[:], cp_, cp_, ALU.mult, r=[("DC", k - 1)], w=["c1"])
                p.tt("dve", c4[:], sp_, sp_, ALU.mult, r=[("DS", k - 1)], w=["c4"])
                p.tt("dve", DC[:, :, k, :], c1[:], c4[:], ALU.subtract, r=["c1", "c4"], w=[("DC", k)])
                p.stt("dve", DS[:, :, k, :], cp_, 2.0, sp_, ALU.mult, ALU.mult, r=[("DC", k - 1), ("DS", k - 1)], w=[("DS", k)])
            p.ts("dve", NDS[:], DS[:], -1.0, ALU.mult, r=[("DS", k) for k in range(12)], w=["NDS"])
            cst = [("DC", k) for k in range(12)] + [("DS", k) for k in range(12)] + ["NDS", "rc"]
            mB = p.sbuf("mBS", [128, 64], F32)
            mJ = p.sbuf("mJS", [128, 64, 8], F32)
            sgn = p.sbuf("sgnS", [128, 2], F32)
            dcol = p.sbuf("dcolS", [128, 8], F32)
            p.dma("act", mB[:], D["s5_maskB"][:, :], w=["mB"])
            p.dma("act", mJ[:], D["s5_maskJ"][:, :, :], w=["mJ"])
            p.dma("act", sgn[:], D["s5_sgn"][:, :], w=["sgn"])
            p.dma("act", dcol[:], D["s5_d"][:, :], w=["dcol"])
            Ct = p.sbuf("CtS", [128, SEQ], F32)
            St = p.sbuf("StS", [128, SEQ], F32)
            wt = p.sbuf("wtS", [128, SEQ], F32)
            zz = p.sbuf("zzS", [128, SEQ], F32)
            tk_ = p.sbuf("tkS", [128, HALF], F32)
            hTc = Rot(p, "hTcS", [128, SEQ], BF16, 1)
            tw = Rot(p, "twS", [128, 512], F32, 2)
            P1 = Rot(p, "P1S", [128, 512], BF16, 2)
            P2 = Rot(p, "P2S", [128, 512], BF16, 2)
            yv = Rot(p, "yvS", [128, 512], F32, 2)
            R = {n: p.sbuf("R%sS" % n, [128, 8, 64], F32) for n in
                 ("are", "aim", "bre", "bim", "ar", "th", "sn", "cs", "t1", "t2", "x", "y", "kr", "ki", "s1", "s2")}
            ldt = p.sbuf("ldtS", [128, 8], F32)
            c1s = p.sbuf("c1sS", [128, 8, 16], F32)
            c2s = p.sbuf("c2sS", [128, 8, 16], F32)
            tmpC = p.sbuf("tmpCS", [128, 8, 8, 16], F32)
            PRM = {(dd, n): p.sbuf("prm%s%d" % (n, dd), [128, 8, 128], BF16) for dd in range(2) for n in ("B", "Bs", "C1", "C2")}
            pbu = Rot(p, "pbuS", [128, 512], F32, 2, psum=True)
            pbs = Rot(p, "pbsS", [128, 512], F32, 2, psum=True)
            yacc = [p.psum("yaccS%d" % i, [128, 512], F32) for i in range(4)]
            for cc in range(8):
                hc, hck = hTc.next()
                p.dma("sp", hc[:], hTd[:, cc, :], w=[hck])
                gs = slice(cc * 8, (cc + 1) * 8)
                for dd in range(2):
                    for n, src in (("are", "s5_are"), ("aim", "s5_aim"), ("bre", "s5_bre"), ("bim", "s5_bim")):
                        p.dma("act", R[n][:], D[src][dd ^ vsw, :, gs, :], w=["R" + n])
                    p.dma("act", ldt[:], D["s5_ldt"][dd ^ vsw, :, gs], w=["ldt"])
                    p.dma("act", c1s[:], D["s5_c1"][dd ^ vsw, :, gs, :], w=["c1s"])
                    p.dma("act", c2s[:], D["s5_c2"][dd ^ vsw, :, gs, :], w=["c2s"])
                    p.act(ldt[:], ldt[:], ACT.Exp, r=["ldt"], w=["ldt"])
                    dtb = ldt[:].unsqueeze(2).to_broadcast([128, 8, 64])
                    p.tt("dve", R["ar"][:], R["are"][:], dtb, ALU.mult, r=["Rare", "ldt"], w=["Rar"])
                    p.tt("dve", R["th"][:], R["aim"][:], dtb, ALU.mult, r=["Raim", "ldt"], w=["Rth"])
                    p.act(R["ar"][:], R["ar"][:], ACT.Exp, r=["Rar"], w=["Rar"])
                    _sincos(p, R["th"][:], R["sn"][:], R["cs"][:], R["s1"][:], R["s2"][:], ["Rth"], "scR")
                    p.tt("dve", R["x"][:], R["ar"][:], R["cs"][:], ALU.mult, r=["Rar", "scR1"], w=["Rx"])
                    p.ts("dve", R["x"][:], R["x"][:], -1.0, ALU.add, r=["Rx"], w=["Rx"])
                    p.tt("dve", R["y"][:], R["ar"][:], R["sn"][:], ALU.mult, r=["Rar", "scR0"], w=["Ry"])
                    p.tt("dve", R["t1"][:], R["are"][:], R["are"][:], ALU.mult, r=["Rare"], w=["Rt1"])
                    p.tt("dve", R["t2"][:], R["aim"][:], R["aim"][:], ALU.mult, r=["Raim"], w=["Rt2"])
                    p.tt("dve", R["t1"][:], R["t1"][:], R["t2"][:], ALU.add, r=["Rt1", "Rt2"], w=["Rt1"])
                    p.op("dve", (lambda e: e.reciprocal(out=R["t1"][:], in_=R["t1"][:])), r=["Rt1"], w=["Rt1"])
                    p.tt("dve", R["kr"][:], R["x"][:], R["are"][:], ALU.mult, r=["Rx", "Rare"], w=["Rkr"])
                    p.tt("dve", R["t2"][:], R["y"][:], R["aim"][:], ALU.mult, r=["Ry", "Raim"], w=["Rt2"])
                    p.tt("dve", R["kr"][:], R["kr"][:], R["t2"][:], ALU.add, r=["Rkr", "Rt2"], w=["Rkr"])
                    p.tt("dve", R["kr"][:], R["kr"][:], R["t1"][:], ALU.mult, r=["Rkr", "Rt1"], w=["Rkr"])
                    p.tt("dve", R["ki"][:], R["y"][:], R["are"][:], ALU.mult, r=["Ry", "Rare"], w=["Rki"])
                    p.tt("dve", R["t2"][:], R["x"][:], R["aim"][:], ALU.mult, r=["Rx", "Raim"], w=["Rt2"])
                    p.tt("dve", R["ki"][:], R["ki"][:], R["t2"][:], ALU.subtract, r=["Rki", "Rt2"], w=["Rki"])
                    p.tt("dve", R["ki"][:], R["ki"][:], R["t1"][:], ALU.mult, r=["Rki", "Rt1"], w=["Rki"])
                    p.tt("dve", R["x"][:], R["kr"][:], R["bre"][:], ALU.mult, r=["Rkr", "Rbre"], w=["Rx"])
                    p.tt("dve", R["t2"][:], R["ki"][:], R["bim"][:], ALU.mult, r=["Rki", "Rbim"], w=["Rt2"])
                    p.tt("dve", R["x"][:], R["x"][:], R["t2"][:], ALU.subtract, r=["Rx", "Rt2"], w=["Rx"])
                    p.tt("dve", R["y"][:], R["kr"][:], R["bim"][:], ALU.mult, r=["Rkr", "Rbim"], w=["Ry"])
                    p.tt("dve", R["t2"][:], R["ki"][:], R["bre"][:], ALU.mult, r=["Rki", "Rbre"], w=["Rt2"])
                    p.tt("dve", R["y"][:], R["y"][:], R["t2"][:], ALU.add, r=["Ry", "Rt2"], w=["Ry"])
                    mBb = mB[:, gs].unsqueeze(2).to_broadcast([128, 8, 64])
                    Bp, Bs = PRM[(dd, "B")], PRM[(dd, "Bs")]
                    kB, kBs = ("prm", dd, "B"), ("prm", dd, "Bs")
                    p.tt("dve", Bp[:, :, 0:64], R["x"][:], mBb, ALU.mult, r=["Rx", "mB"], w=[(kB, 0)])
                    p.tt("dve", Bp[:, :, 64:128], R["y"][:], mBb, ALU.mult, r=["Ry", "mB"], w=[(kB, 1)])
                    p.tt("dve", Bs[:, :, 0:64], R["y"][:], mBb, ALU.mult, r=["Ry", "mB"], w=[(kBs, 0)])
                    p.stt("dve", Bs[:, :, 64:128], R["x"][:], -1.0, mBb, ALU.mult, ALU.mult, r=["Rx", "mB"], w=[(kBs, 1)])
                    mJb = mJ[:, gs, :].unsqueeze(3).to_broadcast([128, 8, 8, 16])
                    for n, cs_, col in (("C1", c1s, 0), ("C2", c2s, 1)):
                        p.tt("dve", tmpC[:], cs_[:].unsqueeze(2).to_broadcast([128, 8, 8, 16]), mJb, ALU.mult,
                             r=["c1s", "c2s", "mJ"], w=["tmpC"])
                        p.ts("dve", PRM[(dd, n)][:].rearrange("p g (j c) -> p g j c", j=8), tmpC[:], sgn[:, col:col + 1], ALU.mult,
                             r=["tmpC", "sgn"], w=[("prm", dd, n)])
                first = True
                for dd in range(2):
                    T = HALF if dd == 0 else SEQ
                    nst = 11 if dd == 0 else 12
                    for g8 in range(8):
                        g = cc * 8 + g8
                        p.memset("pool", Ct[:, 0:1], 1.0, w=["Ct", ("Ct", 0)])
                        p.memset("pool", St[:, 0:1], 0.0, w=["St", ("St", 0)])
                        for k in range(nst):
                            m = 1 << k
                            dc, ds, nds = DC[:, dd, k, g:g + 1], DS[:, dd, k, g:g + 1], NDS[:, dd, k, g:g + 1]
                            ctlo = [("Ct", q) for q in range(k + 1)]
                            stlo = [("St", q) for q in range(k + 1)]
                            p.ts("dve", tk_[:, 0:m], Ct[:, 0:m], dc, ALU.mult, r=ctlo + cst, w=["tk"])
                            p.ts("dve", zz[:, 0:m], St[:, 0:m], dc, ALU.mult, r=stlo, w=["zz"])
                            last_ = (k == nst - 1)
                            p.stt("dve", Ct[:, m:2 * m], St[:, 0:m], nds, tk_[:, 0:m], ALU.mult, ALU.add, r=stlo + ["tk"],
                                  w=[("Ct", k + 1)] + (["Ct"] if last_ else []))
                            p.stt("dve", St[:, m:2 * m], Ct[:, 0:m], ds, zz[:, 0:m], ALU.mult, ALU.add, r=ctlo + ["zz"],
                                  w=[("St", k + 1)] + (["St"] if last_ else []))
                        kB, kBs = ("prm", dd, "B"), ("prm", dd, "Bs")
                        for blk in range(T // 512):
                            j0 = blk * 512
                            pb, pbk = pbu.next()
                            ps_, psk = pbs.next()
                            p.mm(pb[:], PRM[(dd, "B")][:, g8, :], hc[:, j0:j0 + 512], r=[(kB, 0), (kB, 1), hck], w=[pbk])
                            p.mm(ps_[:], PRM[(dd, "Bs")][:, g8, :], hc[:, j0:j0 + 512], r=[(kBs, 0), (kBs, 1), hck], w=[psk])
                            if dd == 0:
                                tsl = slice(j0, j0 + 512)
                            else:
                                a = SEQ - 1 - j0
                                b = a - 512
                                tsl = slice(a, b if b >= 0 else None, -1)
                            t_, tk2 = tw.next()
                            p.tt("dve", t_[:], Ct[:, tsl], pb[:], ALU.mult, r=["Ct", pbk], w=[tk2])
                            p.tt("dve", wt[:, tsl], St[:, tsl], ps_[:], ALU.mult, r=["St", psk], w=[("wt", blk)])
                            p.tt("pool", wt[:, tsl], wt[:, tsl], t_[:], ALU.add, r=[("wt", blk), tk2], w=[("wt", blk)])
                        wk_ = [("wt", blk) for blk in range(T // 512)]
                        p.op("dve", (lambda e, T=T, dd=dd, g=g: e.tensor_tensor_scan(
                            out=zz[:, 0:T], data0=rc[:, dd, g:g + 1].to_broadcast([128, T]), data1=wt[:, 0:T],
                            initial=0.0, op0=ALU.mult, op1=ALU.add)), r=wk_ + ["rc"], w=["zz"])
                        for blk in range(4):
                            j0 = blk * 512
                            if dd == 0:
                                tsl = slice(j0, j0 + 512)
                            else:
                                a = SEQ - 1 - j0
                                tsl = slice(a, a - 512, -1)
                            p1, p1k = P1.next()
                            p2, p2k = P2.next()
                            p.tt("dve", p1[:], Ct[:, tsl], zz[:, tsl], ALU.mult, r=["Ct", "zz"], w=[p1k])
                            p.tt("pool", p2[:], St[:, tsl], zz[:, tsl], ALU.mult, r=["St", "zz"], w=[p2k])
                            last = (dd == 1 and g8 == 7)
                            p.mm(yacc[blk][:], PRM[(dd, "C1")][:, g8, :], p1[:], start=first, stop=False,
                                 r=[("prm", dd, "C1"), p1k], w=[("yacc", blk)])
                            p.mm(yacc[blk][:], PRM[(dd, "C2")][:, g8, :], p2[:], start=False, stop=last,
                                 r=[("prm", dd, "C2"), p2k], w=[("yacc", blk)])
                        first = False
                for blk in range(4):
                    y_, yk = yv.next()
                    p.stt("dve", y_[:], hc[:, blk * 512:(blk + 1) * 512], dcol[:, cc:cc + 1], yacc[blk][:], ALU.mult, ALU.add,
                          r=[hck, "dcol", ("yacc", blk)], w=[yk])
                    p.act(ygT[:, cc, blk * 512:(blk + 1) * 512], y_[:], ACT.Gelu_apprx_tanh, r=[yk], w=[("ygT", cc, blk)])
        with p.scope():
            wg = p.sbuf("wgS", [128, 8, 1024], BF16)
            wo = p.sbuf("woS", [128, 8, 1024], BF16)
            p.dma("pool", wg[:], D["w_glu"].rearrange("(c p) n -> p c n", p=128), w=["wg"])
            p.dma("pool", wo[:], D["w_so"].rearrange("(c p) n -> p c n", p=128), w=["wo"])
            y2T = p.sbuf("y2TS", [128, 8, HALF], BF16)
            pz = Rot(p, "pzS", [128, 512], F32, 2, psum=True)
            sg = Rot(p, "sgS", [128, 512], F32, 2)
            for oc in range(8):
                for blk in range(4):
                    ps, psk = pz.next()
                    for k in range(8):
                        p.mm(ps[:], wg[:, k, oc * 128:(oc + 1) * 128], ygT[:, k, blk * 512:(blk + 1) * 512],
                             start=(k == 0), stop=(k == 7), r=["wg"], w=[psk])
                    s_, sk = sg.next()
                    p.act(s_[:], ps[:], ACT.Sigmoid, r=[psk], w=[sk])
                    p.tt("dve", y2T[:, oc, blk * 512:(blk + 1) * 512], ygT[:, oc, blk * 512:(blk + 1) * 512], s_[:], ALU.mult,
                         r=[sk], w=[("y2T", oc, blk)])
            pY = Rot(p, "pYS", [128, 512], F32, 2, psum=True)
            yo = Rot(p, "yoS", [128, 1024], F32, 2)
            for qt in range(16):
                y_, yk = yo.next()
                for hh in range(2):
                    py, pyk = pY.next()
                    for k in range(8):
                        p.mm(py[:], y2T[:, k, qt * 128:(qt + 1) * 128], wo[:, k, hh * 512:(hh + 1) * 512],
                             start=(k == 0), stop=(k == 7), r=["wo"] + [("y2T", k, qt // 4)], w=[pyk])
                    p.copy("act" if hh == 0 else "dve", y_[:, hh * 512:(hh + 1) * 512], py[:], r=[pyk], w=[(yk, hh)])
                p.dma("sp", ymix[qt * 128:(qt + 1) * 128, :], y_[:], r=[(yk, 0), (yk, 1)], w=[("ymix", qt)])


def s5_inputs(inp, j, hf):
    f = np.float32
    order = [0, 1] if hf == 0 else [1, 0]
    are = inp["s5_a_re"][j][order]
    aim = inp["s5_a_im"][j][order]
    ldt = inp["s5_log_dt"][j][order]
    bre = inp["s5_b_re"][j][order]
    bim = inp["s5_b_im"][j][order]
    cre = inp["s5_c_re"][j][order]
    cim = inp["s5_c_im"][j][order]
    d = {}
    d["s5_are"] = np.ascontiguousarray(np.broadcast_to(are[:, None], (2, 128, 64, 64))).astype(f)
    d["s5_aim"] = np.ascontiguousarray(np.broadcast_to(aim[:, None], (2, 128, 64, 64))).astype(f)
    d["s5_ldt"] = np.ascontiguousarray(np.broadcast_to(ldt[:, None], (2, 128, 64))).astype(f)
    bt = bre.transpose(0, 3, 1, 2)
    d["s5_bre"] = np.ascontiguousarray(np.tile(bt, (1, 8, 1, 1))).astype(f)
    bt = bim.transpose(0, 3, 1, 2)
    d["s5_bim"] = np.ascontiguousarray(np.tile(bt, (1, 8, 1, 1))).astype(f)
    crt = cre.transpose(0, 3, 1, 2)
    cit = cim.transpose(0, 3, 1, 2)
    d["s5_c1"] = np.ascontiguousarray(np.concatenate([crt, cit], axis=1)).astype(f)
    d["s5_c2"] = np.ascontiguousarray(np.concatenate([cit, crt], axis=1)).astype(f)
    col = np.stack([are.transpose(0, 2, 1), aim.transpose(0, 2, 1),
                    np.broadcast_to(ldt[:, None, :], (2, 64, 64))], axis=2)
    d["s5_colp"] = np.ascontiguousarray(np.concatenate([col, col], axis=1)).astype(f)
    g = np.arange(64)
    jj = np.arange(8)
    mB = (g[None, :] % 8 == (np.arange(128) // 16)[:, None]).astype(f)
    d["s5_maskB"] = mB
    mJ = (g[:, None] % 8 == jj[None, :]).astype(f)
    d["s5_maskJ"] = np.ascontiguousarray(np.broadcast_to(mJ[None], (128, 64, 8))).astype(f)
    sgn = np.ones((128, 2), f)
    sgn[64:, 0] = -1.0
    sgn[:, 1] = -1.0
    d["s5_sgn"] = sgn
    d["s5_d"] = np.ascontiguousarray(inp["s5_d"][j].reshape(8, 128).T).astype(f)
    d["w_glu"] = inp["s5_w_glu"][j]
    d["w_so"] = inp["s5_w_out"][j]
    return d


LAYER_INPUTS = {
    "common": [("cT", [128, 8], F32), ("ada_w", [D_MODEL, 6 * D_MODEL], F32), ("ada_b", [1, 6 * D_MODEL], F32),
               ("pln", [4, 128, D_MODEL], F32), ("w_r", [D_MODEL, NEXP], F32), ("b_r", [1, NEXP], F32),
               ("w_gu", [NEXP, D_MODEL, 2 * D_MODEL], F32), ("b_guT", [128, NEXP, 16], F32),
               ("w_down", [NEXP, D_MODEL, D_MODEL], F32), ("b_down", [NEXP, D_MODEL], F32)],
    0: [("dftL", [4, 8, 128, 4, 2, 512], BF16), ("dftC", [128, 2, 2, 256], BF16), ("fnet_w", [D_MODEL, D_MODEL], F32),
        ("fnet_b", [1, D_MODEL], F32)],
    1: [("w_qkv", [D_MODEL, 2048], F32), ("g12", [128, 12, 128], F32), ("rope", [32, 128, 2, 128], F32),
        ("w_o", [D_MODEL, D_MODEL], F32)],
    2: [("s5_are", [2, 128, 64, 64], F32), ("s5_aim", [2, 128, 64, 64], F32), ("s5_ldt", [2, 128, 64], F32),
        ("s5_bre", [2, 128, 64, 64], F32), ("s5_bim", [2, 128, 64, 64], F32), ("s5_c1", [2, 128, 64, 16], F32),
        ("s5_c2", [2, 128, 64, 16], F32), ("s5_colp", [2, 128, 3, 64], F32), ("s5_maskB", [128, 64], F32),
        ("s5_maskJ", [128, 64, 8], F32), ("s5_sgn", [128, 2], F32), ("s5_d", [128, 8], F32),
        ("w_glu", [D_MODEL, D_MODEL], F32), ("w_so", [D_MODEL, D_MODEL], F32)],
    3: [("w_in", [D_MODEL, 5, D_MODEL], F32), ("lbT", [128, 4, 8], F32), ("cmask", [128, HALF], F32),
        ("tri", [64, 2, 64], F32), ("hnorm", [64, 8, 128], F32), ("w_ho", [D_MODEL, D_MODEL], F32)],
}


def phase_local_exchange(c, outs, xps, li):
    p = c.p
    with p.scope():
        a_ = Rot(p, "xa", [128, 1024], F32, 3)
        o_ = Rot(p, "xo", [128, 1024], F32, 3)
        ps = Rot(p, "xps", [128, 512], F32, 4, psum=True)
        for v in range(2):
            for t in range(16):
                src0 = HALF - 128 * (t + 1)
                a, ak = a_.next()
                p.dma("sp", a[:], outs[1 - v][src0:src0 + 128, :], w=[ak])
                o, ok = o_.next()
                for hh in range(2):
                    q, qk = ps.next()
                    p.mm(q[:], c.jrev[:], a[:, hh * 512:(hh + 1) * 512], r=[ak], w=[qk])
                    p.copy("act" if hh == 0 else "dve", o[:, hh * 512:(hh + 1) * 512], q[:], r=[qk], w=[(ok, hh)])
                p.dma("sp", xps[v][HALF + t * 128:HALF + (t + 1) * 128, :], o[:], r=[(ok, 0), (ok, 1)], w=[("xpn", v, t)])
            p.dma("act", xps[v][0:HALF, :], outs[v][:, :], w=[("xpn0", v)])


HF_DEP = {0: ["dftL"], 1: ["rope"], 2: [], 3: []}


def build_fused(depth=DEPTH):
    nc = bass.Bass("TRN2", target_bir_lowering=False)
    c = Ctx()
    c.nc = nc
    Dall = {}
    c.Dall = Dall

    def din(name, shape, dt):
        Dall[name] = nc.dram_tensor(name, list(shape), dt, kind="ExternalInput").ap()
    din("xp_v0", [SEQ, D_MODEL], F32)
    din("xp_v1", [SEQ, D_MODEL], F32)
    for li in range(depth):
        m = li % 4
        for name, shape, dt in LAYER_INPUTS["common"] + LAYER_INPUTS[m]:
            if name in HF_DEP[m]:
                for v in range(2):
                    din("%s_L%d_v%d" % (name, li, v), shape, dt)
            else:
                din("%s_L%d" % (name, li), shape, dt)
    out = nc.dram_tensor("out", [2, HALF, D_MODEL], F32, kind="ExternalOutput").ap()
    c.p = Prog(nc)
    setup_consts(c)
    p = c.p
    c.jrev = p.sbuf("jrev", [128, 128], F32)
    p.memset("dve", c.jrev[:], 0.0, w=["jrev"])
    p.op("pool", (lambda e: e.affine_select(out=c.jrev[:], in_=c.jrev[:], pattern=[[1, 128]], compare_op=ALU.not_equal,
                                            fill=1.0, base=-127, channel_multiplier=1)), r=["jrev"], w=["jrev"])
    p.flush()
    xps = [Dall["xp_v0"], Dall["xp_v1"]]
    for li in range(depth):
        m = li % 4
        last = li == depth - 1
        outs = [out[v] if last else nc.dram_tensor("xown%d_%d" % (li, v), [HALF, D_MODEL], F32).ap() for v in range(2)]
        c.modd = nc.dram_tensor("modd%d" % li, [128, 6 * D_MODEL], F32).ap()
        p.new_epoch()
        for v in range(2):
            D = {}
            for name, shape, dt in LAYER_INPUTS["common"] + LAYER_INPUTS[m]:
                D[name] = Dall["%s_L%d_v%d" % (name, li, v)] if name in HF_DEP[m] else Dall["%s_L%d" % (name, li)]
            D["xp"] = xps[v]
            D["out"] = outs[v]
            c.D = D
            c.vhf = v
            sfx = "%d_%d" % (li, v)
            c.lsuffix = sfx
            c.x1buf = nc.dram_tensor("x1buf" + sfx, [HALF, D_MODEL], F32).ap()
            c.h2T = nc.dram_tensor("h2T" + sfx, [128, 8, HALF], BF16).ap()
            c.gbuf = nc.dram_tensor("gbuf" + sfx, [16, 128, NEXP], F32).ap()
            ymix = nc.dram_tensor("ymix" + sfx, [HALF, D_MODEL], F32).ap()
            if v == 0:
                phase_mod(c)
            if m == 0:
                phase_fnet(c, ymix)
            elif m == 1:
                phase_attn(c, ymix)
            elif m == 2:
                phase_s5(c, ymix)
            else:
                phase_hgrn(c, ymix, li)
            phase_post(c, ymix)
            phase_moe(c)
        if not last:
            nxt = [nc.dram_tensor("xpn%d_%d" % (li, v), [SEQ, D_MODEL], F32).ap() for v in range(2)]
            phase_local_exchange(c, outs, nxt, li)
            xps = nxt
    c.p.finish()
    c.n_ops = c.p.total_ops
    return nc, c


_FUSED = []


def layer_inputs(inp, li, b, hf, cache):
    m, j = li % 4, li // 4
    key = ("common", li, b)
    if key not in cache:
        cache[key] = common_inputs(inp, li, b)
    d = dict(cache[key])
    if m == 0:
        if ("dft", hf) not in cache:
            cache[("dft", hf)] = fnet_tables(hf)
            cache["dftC"] = fnet_ctab()
        d["dftL"] = cache[("dft", hf)]
        d["dftC"] = cache["dftC"]
        d["fnet_w"] = inp["fnet_w_out"][j]
        d["fnet_b"] = inp["fnet_b_out"][j][None, :]
    elif m == 1:
        d["w_qkv"] = inp["attn_w_qkv"][j]
        g = np.concatenate([np.tile(inp["attn_q_norm"][j][None, :], (8, 1)), np.tile(inp["attn_k_norm"][j][None, :], (4, 1))])
        d["g12"] = np.ascontiguousarray(np.broadcast_to(g[None], (128, 12, 128))).astype(np.float32)
        d["rope"] = rope_tables(hf)
        d["w_o"] = inp["attn_w_out"][j]
    elif m == 2:
        d.update(s5_inputs(inp, j, hf))
    else:
        w = inp["hg_w_in"][j].reshape(D_MODEL, 5, D_MODEL)
        if hf == 1:
            w = w[:, [0, 2, 1, 3, 4], :]
        d["w_in"] = np.ascontiguousarray(w)
        d["lbT"] = np.ascontiguousarray(inp["hg_lb"].reshape(4, 8, 128).transpose(2, 0, 1))
        cmk = np.ones((128, HALF), np.float32)
        cmk[:, ::64] = 0.0
        d["cmask"] = cmk
        sidx = np.arange(64)
        d["tri"] = np.stack([(sidx[:, None] <= sidx[None, :]), (sidx[:, None] >= sidx[None, :])], axis=1).astype(np.float32)
        d["hnorm"] = np.ascontiguousarray(np.broadcast_to(inp["hg_norm"][j].reshape(1, 8, 128), (64, 8, 128))).astype(np.float32)
        d["w_ho"] = inp["hg_w_out"][j]
    return d


def kernel_fused(inp, depth=DEPTH):
    if not _FUSED:
        _FUSED.append(build_fused(depth))
    nc, c = _FUSED[0]
    x = np.asarray(inp["x"], dtype=np.float32)
    cache = {}
    in_maps = []
    NB = x.shape[0]
    for b in range(NB):
        d = {"xp_v0": present(x[b], 0), "xp_v1": present(x[b], 1)}
        for li in range(depth):
            m = li % 4
            per_v = [layer_inputs(inp, li, b, v, cache) for v in range(2)] if HF_DEP[m] else None
            base = layer_inputs(inp, li, b, 0, cache)
            for k, v_ in base.items():
                if k in HF_DEP[m]:
                    for v in range(2):
                        d["%s_L%d_v%d" % (k, li, v)] = per_v[v][k]
                else:
                    d["%s_L%d" % (k, li)] = v_
        in_maps.append(d)
    res = run_bass_kernel_spmd(nc, in_maps, core_ids=list(range(NB)))
    out = np.empty_like(x)
    for b in range(NB):
        o = res.results[b]["out"]
        out[b, unpresent_idx(0)] = o[0]
        out[b, unpresent_idx(1)] = o[1]
    return out


def kernel(**inputs):
    inp = {k: np.asarray(v) for k, v in inputs.items()}
    x = np.asarray(inp["x"], dtype=np.float32)
    for li in range(DEPTH):
        x = run_layer(li, x, inp)
    return x
```
